# Optimizing a Trainium2 kernel written in Bass

```python
import math
import jax, jax.numpy as jnp
from jax import lax
import numpy as np

D_MODEL = 1024
BATCH = 4
SEQ = 4096
DEPTH = 1

ATT_HEADS = 8
ATT_KV_HEADS = 2
ATT_HEAD_DIM = 64
ATT_GROUP = ATT_HEADS // ATT_KV_HEADS
WINDOW = 128
BLOCK = 128
ML_HEADS = 4
ML_HEAD_DIM = 128
ML_CHUNK = 64
CONV_WIDTH = 4
MEM_LEN = 256
X_HEADS = 4
X_HEAD_DIM = 128
N_GROUPS = 4
EXPERTS_PER_GROUP = 8
N_EXPERTS = N_GROUPS * EXPERTS_PER_GROUP
TOP_K = 2
D_EXPERT = 512
MOE_BLOCK = 128
EPS = 1e-6

ATT_Q = ATT_HEADS * ATT_HEAD_DIM
ATT_KV = ATT_KV_HEADS * ATT_HEAD_DIM
ML_W = ML_HEADS * ML_HEAD_DIM
MIX_WIDTH = ATT_Q + ML_W
IN_SPLITS = [ATT_Q, ATT_KV, ATT_KV, 2 * ML_W, ML_W, ML_W, 2 * ML_HEADS]
IN_COLS = sum(IN_SPLITS)
IN_OFFSETS = [int(v) for v in np.cumsum(IN_SPLITS)[:-1]]

kernel_name = 'hybrid_swa_mlstm_hmoe'


def rms_norm(x, g):
    xf = x.astype(jnp.float32)
    y = xf * lax.rsqrt(jnp.mean(xf * xf, axis=-1, keepdims=True) + EPS)
    return (y * g.astype(jnp.float32)).astype(x.dtype)


def alibi_slopes(n):
    return jnp.exp2(-8.0 * jnp.arange(1, n + 1, dtype=jnp.float32) / n)


def causal_depthwise_conv(x, w, b):
    c = x.shape[-1]
    y = lax.conv_general_dilated(x, w[:, None, :], window_strides=(1,),
                                 padding=[(CONV_WIDTH - 1, 0)],
                                 dimension_numbers=('NWC', 'WIO', 'NWC'),
                                 feature_group_count=c)
    return y + b


def sliding_window_gqa(q, k, v, sinks):
    bsz, s = q.shape[0], q.shape[1]
    nb = s // BLOCK
    qb = q.reshape(bsz, nb, BLOCK, ATT_KV_HEADS, ATT_GROUP, ATT_HEAD_DIM)

    def with_prev(t):
        t = t.reshape(bsz, nb, BLOCK, ATT_KV_HEADS, ATT_HEAD_DIM)
        prev = jnp.pad(t[:, :-1], ((0, 0), (1, 0), (0, 0), (0, 0), (0, 0)))
        return jnp.concatenate([prev, t], axis=2)

    kk, vv = with_prev(k), with_prev(v)
    scores = jnp.einsum('bnqkgd,bnskd->bkgnqs', qb, kk).astype(jnp.float32)
    scores = scores * (1.0 / math.sqrt(ATT_HEAD_DIM))
    dist = (jnp.arange(BLOCK)[:, None] + BLOCK) - jnp.arange(2 * BLOCK)[None, :]
    in_band = (dist >= 0) & (dist < WINDOW)
    pad_key = (jnp.arange(nb)[:, None, None] == 0) & (jnp.arange(2 * BLOCK)[None, None, :] < BLOCK)
    valid = in_band[None] & ~pad_key
    slopes = alibi_slopes(ATT_HEADS).reshape(ATT_KV_HEADS, ATT_GROUP)[:, :, None, None, None]
    logits = scores - slopes * dist.astype(jnp.float32)
    logits = jnp.where(valid, logits, -jnp.inf)
    sink = sinks.astype(jnp.float32).reshape(ATT_KV_HEADS, ATT_GROUP)[:, :, None, None, None]
    mx = jnp.maximum(logits.max(axis=-1, keepdims=True), sink)
    e = jnp.exp(logits - mx)
    p = e / (e.sum(axis=-1, keepdims=True) + jnp.exp(sink - mx))
    out = jnp.einsum('bkgnqs,bnskd->bnqkgd', p.astype(v.dtype), vv)
    return out.reshape(bsz, s, ATT_Q)


def mlstm_chunk_step(carry, inp):
    c_st, n_st, m_st = carry
    q, k, v, ig, lf = inp
    L = q.shape[2]
    b = jnp.cumsum(lf, axis=-1)
    causal = jnp.tril(jnp.ones((L, L), dtype=bool))
    log_d = jnp.where(causal, b[..., :, None] - b[..., None, :] + ig[..., None, :], -jnp.inf)
    m_inter = b + m_st[..., None]
    m_t = jnp.maximum(m_inter, log_d.max(axis=-1))
    s = jnp.einsum('bhtd,bhsd->bhts', q, k) * jnp.exp(log_d - m_t[..., None])
    a_inter = jnp.exp(m_inter - m_t)
    num = jnp.einsum('bhts,bhsd->bhtd', s, v) + a_inter[..., None] * jnp.einsum('bhtk,bhkv->bhtv', q, c_st)
    den = s.sum(axis=-1) + a_inter * jnp.einsum('bhtk,bhk->bht', q, n_st)
    h = num / jnp.maximum(jnp.abs(den), jnp.exp(-m_t))[..., None]
    b_last = b[..., -1]
    log_w = b_last[..., None] - b + ig
    m_new = jnp.maximum(b_last + m_st, log_w.max(axis=-1))
    w = jnp.exp(log_w - m_new[..., None])
    decay = jnp.exp(b_last + m_st - m_new)
    c_new = decay[..., None, None] * c_st + jnp.einsum('bhsk,bhsv->bhkv', k * w[..., None], v)
    n_new = decay[..., None] * n_st + jnp.einsum('bhs,bhsk->bhk', w, k)
    return (c_new, n_new, m_new), h


def mlstm(q, k, v, i_pre, f_pre):
    bsz, s, nh, dh = q.shape
    nc = s // ML_CHUNK
    f32 = jnp.float32
    def chunks(t):
        return t.astype(f32).reshape(bsz, nc, ML_CHUNK, nh, dh).transpose(1, 0, 3, 2, 4)
    def gchunks(t):
        return t.reshape(bsz, nc, ML_CHUNK, nh).transpose(1, 0, 3, 2)
    qc = chunks(q)
    kc = chunks(k) * (1.0 / math.sqrt(dh))
    vc = chunks(v)
    igc = gchunks(i_pre.astype(f32))
    lfc = gchunks(jax.nn.log_sigmoid(f_pre.astype(f32)))
    init = (jnp.zeros((bsz, nh, dh, dh), f32), jnp.zeros((bsz, nh, dh), f32), jnp.zeros((bsz, nh), f32))
    _, hs = lax.scan(mlstm_chunk_step, init, (qc, kc, vc, igc, lfc))
    return hs.transpose(1, 0, 3, 2, 4).reshape(bsz, s, nh, dh)


def memory_cross_attention(xn, memn, w_cq, w_ckv, w_co):
    bsz, s, _ = xn.shape
    q = (xn @ w_cq).reshape(bsz, s, X_HEADS, X_HEAD_DIM)
    kv = (memn @ w_ckv).reshape(bsz, memn.shape[1], 2, X_HEADS, X_HEAD_DIM)
    k, v = kv[:, :, 0], kv[:, :, 1]
    sc = jnp.einsum('bshd,bmhd->bhsm', q, k).astype(jnp.float32) * (1.0 / math.sqrt(X_HEAD_DIM))
    p = jax.nn.softmax(sc, axis=-1)
    o = jnp.einsum('bhsm,bmhd->bshd', p.astype(v.dtype), v).reshape(bsz, s, X_HEADS * X_HEAD_DIM)
    return o @ w_co


def hierarchical_moe(xn, w_rg, b_rg, w_re, b_re, w_g, w_u, w_d):
    bsz, s, d = xn.shape
    t = bsz * s
    xt = xn.reshape(t, d)
    g_logits = (xt @ w_rg).astype(jnp.float32) + b_rg.astype(jnp.float32)
    g_prob = jax.nn.softmax(g_logits, axis=-1)
    g_sel = jnp.argmax(g_logits, axis=-1)
    g_w = jnp.take_along_axis(g_prob, g_sel[:, None], axis=-1)
    e_logits = ((xt @ w_re).astype(jnp.float32) + b_re.astype(jnp.float32)).reshape(t, N_GROUPS, EXPERTS_PER_GROUP)
    e_in = jnp.take_along_axis(e_logits, g_sel[:, None, None], axis=1)[:, 0]
    top_v, top_i = lax.top_k(e_in, TOP_K)
    top_p = jax.nn.softmax(top_v, axis=-1) * g_w
    expert_idx = (g_sel[:, None] * EXPERTS_PER_GROUP + top_i).reshape(-1)
    gates = top_p.reshape(-1)
    n_assign = t * TOP_K
    token_idx = jnp.arange(n_assign) // TOP_K
    order = jnp.argsort(expert_idx)
    sorted_e = expert_idx[order]
    counts = jnp.bincount(expert_idx, length=N_EXPERTS)
    padded = (counts + MOE_BLOCK - 1) // MOE_BLOCK * MOE_BLOCK
    pad_end = jnp.cumsum(padded)
    pad_start = pad_end - padded
    start = jnp.cumsum(counts) - counts
    dest = pad_start[sorted_e] + (jnp.arange(n_assign) - start[sorted_e])
    n_blocks = -(-n_assign // MOE_BLOCK) + N_EXPERTS
    x_pad = jnp.zeros((n_blocks * MOE_BLOCK, d), xt.dtype).at[dest].set(xt[token_idx[order]])
    block_e = jnp.minimum(jnp.searchsorted(pad_end, jnp.arange(n_blocks) * MOE_BLOCK, side='right'), N_EXPERTS - 1)

    def expert_block(args):
        xb, e = args
        hb = jax.nn.silu(xb @ w_g[e]) * (xb @ w_u[e])
        return hb @ w_d[e]

    y_pad = lax.map(expert_block, (x_pad.reshape(n_blocks, MOE_BLOCK, d), block_e)).reshape(-1, d)
    y = y_pad[dest] * gates[order][:, None].astype(y_pad.dtype)
    out = jnp.zeros((t, d), xt.dtype).at[token_idx[order]].add(y)
    return out.reshape(bsz, s, d)


def setup_inputs(seed: int = 0) -> dict:
    key = jax.random.key(seed)
    ks = iter(jax.random.split(key, 40))
    f32 = jnp.float32

    def nrm(shape, scale):
        return jax.random.normal(next(ks), shape, f32) * scale

    def gain(shape):
        return 1.0 + nrm(shape, 0.02)

    L = DEPTH
    x = nrm((BATCH, SEQ, D_MODEL), 1.0)
    mem = nrm((BATCH, MEM_LEN, D_MODEL), 1.0)
    b_i = nrm((L, ML_HEADS), 0.1)
    b_f = jnp.linspace(3.0, 6.0, ML_HEADS, dtype=f32)[None, :] + nrm((L, ML_HEADS), 0.01)
    return {
        'x': x,
        'mem': mem,
        'norm_mix': gain((L, D_MODEL)),
        'w_in': nrm((L, D_MODEL, IN_COLS), D_MODEL ** -0.5),
        'b_gates': jnp.concatenate([b_i, b_f], axis=-1),
        'conv_w': nrm((L, CONV_WIDTH, 2 * ML_W), CONV_WIDTH ** -0.5),
        'conv_b': nrm((L, 2 * ML_W), 0.01),
        'att_sinks': nrm((L, ATT_HEADS), 0.5),
        'norm_att_out': gain((L, ATT_Q)),
        'norm_ml_out': gain((L, ML_W)),
        'w_out': nrm((L, MIX_WIDTH, D_MODEL), MIX_WIDTH ** -0.5),
        'norm_cross': gain((L, D_MODEL)),
        'norm_mem': gain((L, D_MODEL)),
        'w_cq': nrm((L, D_MODEL, X_HEADS * X_HEAD_DIM), D_MODEL ** -0.5),
        'w_ckv': nrm((L, D_MODEL, 2 * X_HEADS * X_HEAD_DIM), D_MODEL ** -0.5),
        'w_co': nrm((L, X_HEADS * X_HEAD_DIM, D_MODEL), (X_HEADS * X_HEAD_DIM) ** -0.5),
        'norm_ffn': gain((L, D_MODEL)),
        'w_router_group': nrm((L, D_MODEL, N_GROUPS), D_MODEL ** -0.5),
        'b_router_group': nrm((L, N_GROUPS), 0.01),
        'w_router_expert': nrm((L, D_MODEL, N_EXPERTS), D_MODEL ** -0.5),
        'b_router_expert': nrm((L, N_EXPERTS), 0.01),
        'w_e_gate': nrm((L, N_EXPERTS, D_MODEL, D_EXPERT), D_MODEL ** -0.5),
        'w_e_up': nrm((L, N_EXPERTS, D_MODEL, D_EXPERT), D_MODEL ** -0.5),
        'w_e_down': nrm((L, N_EXPERTS, D_EXPERT, D_MODEL), D_EXPERT ** -0.5),
        'norm_final': gain((D_MODEL,)),
    }


def reference(x, mem, norm_mix, w_in, b_gates, conv_w, conv_b, att_sinks, norm_att_out,
              norm_ml_out, w_out, norm_cross, norm_mem, w_cq, w_ckv, w_co, norm_ffn,
              w_router_group, b_router_group, w_router_expert, b_router_expert,
              w_e_gate, w_e_up, w_e_down, norm_final):
    bsz, s, _ = x.shape
    for l in range(DEPTH):
        h = rms_norm(x, norm_mix[l])
        proj = h @ w_in[l]
        q_a, k_a, v_a, qk_m, v_m, o_m, g_m = jnp.split(proj, IN_OFFSETS, axis=-1)
        att = sliding_window_gqa(q_a.reshape(bsz, s, ATT_HEADS, ATT_HEAD_DIM),
                                 k_a.reshape(bsz, s, ATT_KV_HEADS, ATT_HEAD_DIM),
                                 v_a.reshape(bsz, s, ATT_KV_HEADS, ATT_HEAD_DIM),
                                 att_sinks[l])
        att = rms_norm(att, norm_att_out[l])
        qk_m = jax.nn.silu(causal_depthwise_conv(qk_m, conv_w[l], conv_b[l]))
        q_m, k_m = jnp.split(qk_m, 2, axis=-1)
        g_m = g_m + b_gates[l]
        i_pre, f_pre = g_m[..., :ML_HEADS], g_m[..., ML_HEADS:]
        hm = mlstm(q_m.reshape(bsz, s, ML_HEADS, ML_HEAD_DIM),
                   k_m.reshape(bsz, s, ML_HEADS, ML_HEAD_DIM),
                   v_m.reshape(bsz, s, ML_HEADS, ML_HEAD_DIM), i_pre, f_pre)
        hm = jax.nn.sigmoid(o_m.astype(jnp.float32)).reshape(bsz, s, ML_HEADS, ML_HEAD_DIM) * hm
        hm = rms_norm(hm.astype(x.dtype), norm_ml_out[l].reshape(ML_HEADS, ML_HEAD_DIM)).reshape(bsz, s, ML_W)
        x = x + jnp.concatenate([att, hm], axis=-1) @ w_out[l]
        x = x + memory_cross_attention(rms_norm(x, norm_cross[l]), rms_norm(mem, norm_mem[l]),
                                       w_cq[l], w_ckv[l], w_co[l])
        x = x + hierarchical_moe(rms_norm(x, norm_ffn[l]), w_router_group[l], b_router_group[l],
                                 w_router_expert[l], b_router_expert[l],
                                 w_e_gate[l], w_e_up[l], w_e_down[l])
    return rms_norm(x, norm_final)
```

```python
import math
import os
from contextlib import ExitStack

import numpy as np
import concourse.bass as bass
import concourse.mybir as mybir
from concourse.bass_utils import run_bass_kernel_spmd

F32 = mybir.dt.float32
BF16 = mybir.dt.bfloat16
I32 = mybir.dt.int32
AF = mybir.ActivationFunctionType
ALU = mybir.AluOpType
AX = mybir.AxisListType

ENGS = ["pe", "act", "dve", "pool", "sp"]
NT = 16
NG = 8
GT = 256
NE = 32
CAP = 256
EPS = 1e-6
LN_SQRT_DH = 0.5 * math.log(128.0)
EPOCH = 400


class Op:
    __slots__ = ("eng", "emit", "deps", "is_dma", "semkey", "count", "signal", "nosync", "depcnt", "idx")


class Sched:
    def __init__(self):
        self.ops = {e: [] for e in ENGS}
        self.last_w = {}
        self.readers = {}
        self.dma_cnt = {}
        self.dma_last = {}
        self.nsig = {}

    def add(self, eng, emit, reads=(), writes=(), dma=None, nosync=False):
        op = Op()
        op.eng = eng
        op.emit = emit
        op.is_dma = dma is not None
        op.semkey = dma
        op.signal = False
        op.count = 0
        op.nosync = nosync
        op.idx = len(self.ops[eng])
        deps = []
        for r in reads:
            w = self.last_w.get(r)
            if w is not None:
                deps.append(w)
        for k in writes:
            w = self.last_w.get(k)
            if w is not None:
                deps.append(w)
            deps.extend(self.readers.get(k, ()))
        own_before = 0
        if op.is_dma:
            p = self.dma_last.get(dma)
            if p is not None and not nosync:
                deps.append(p)
            self.dma_last[dma] = op
            own_before = self.dma_cnt.get(dma, 0)
            self.dma_cnt[dma] = own_before + 16
            op.count = self.dma_cnt[dma]
        seen = set()
        od = []
        for d in deps:
            if id(d) in seen or d is op:
                continue
            seen.add(id(d))
            if (not d.is_dma) and (not op.is_dma) and d.eng == "pe" and eng == "pe":
                continue
            od.append(d)
        latest = {}
        for d in od:
            if not d.is_dma:
                if d.eng not in latest or d.idx > latest[d.eng].idx:
                    latest[d.eng] = d
        od = [d for d in od if d.is_dma or latest[d.eng] is d]
        for d in od:
            if not d.is_dma:
                d.signal = True
        op.deps = od
        op.depcnt = [((own_before if (op.is_dma and d.semkey == op.semkey) else self.dma_cnt[d.semkey]) if (d.is_dma and d.nosync) else None) for d in od]
        for r in reads:
            self.readers.setdefault(r, []).append(op)
        for k in writes:
            self.last_w[k] = op
            self.readers[k] = []
        self.ops[eng].append(op)
        return op

    def finalize(self):
        for e in ENGS:
            c = 0
            for op in self.ops[e]:
                if op.is_dma:
                    continue
                if op.signal:
                    op.count = c
                    c += 1
            self.nsig[e] = c

    def emit_engine(self, eng, handle, sems, dma_sems):
        seen = {}
        for op in self.ops[eng]:
            need = {}
            for d, dc in zip(op.deps, op.depcnt):
                if d.is_dma:
                    key = ("d", d.semkey)
                    cnt_ = d.count if dc is None else dc
                else:
                    key = ("e", d.eng)
                    cnt_ = d.count + 1
                if cnt_ > need.get(key, 0):
                    need[key] = cnt_
            for key, cnt in need.items():
                if seen.get(key, 0) >= cnt:
                    continue
                seen[key] = cnt
                if key[0] == "d":
                    handle.wait_ge(dma_sems[key[1]], cnt)
                else:
                    ep, v = (cnt - 1) // EPOCH, (cnt - 1) % EPOCH + 1
                    handle.wait_ge(sems[key[1]][ep], v)
            if op.emit is None:
                continue
            ins = op.emit(handle)
            if op.is_dma:
                ins.then_inc(dma_sems[op.semkey], 16)
            elif op.signal:
                ins.then_inc(sems[eng][op.count // EPOCH], 1)


def build(dbg=False, stage=9):
    nc = bass.Bass("TRN2", target_bir_lowering=False)
    S = Sched()

    def din(name, shape, dt=F32):
        return nc.dram_tensor(name, shape, dt, kind="ExternalInput").ap()

    xo = din("xo", [2048, 1024])
    xp = din("xp", [2048, 1024])
    memb = din("memb", [256, 1024])
    w_in = din("w_in", [1024, 2824])
    w_out = din("w_out", [1024, 1024])
    w_cq = din("w_cq", [1024, 512])
    w_ckv = din("w_ckv", [1024, 1024])
    w_co = din("w_co", [512, 1024])
    w_r = din("w_r", [1024, 36])
    NEd = NE if stage >= 5 else 1
    w_eg = din("w_eg", [NEd, 1024, 512])
    w_eu = din("w_eu", [NEd, 1024, 512])
    w_ed = din("w_ed", [NEd, 512, 1024])
    bcd = din("bcd", [128, 6 * 1024 + 128])
    ppd = din("ppd", [128, 128])
    cmd = din("cmd", [128, 5, 128])
    outd = nc.dram_tensor("out", [2048, 1024], F32, kind="ExternalOutput").ap()
    xs_d = nc.dram_tensor("xs_d", [NE * CAP + 128, 1024], BF16, kind="Internal").ap()
    ys_d = nc.dram_tensor("ys_d", [NE * CAP + 128, 1024], F32, kind="Internal").ap()
    x2_d = nc.dram_tensor("x2_d", [2048, 1024], F32, kind="Internal").ap()
    dbgd = {}
    if dbg:
        for nm, sh in [("d_att", [2048, 512]), ("d_hm", [2048, 512]), ("d_x1", [2048, 1024]),
                       ("d_x2", [2048, 1024]), ("d_lg", [2048, 36]), ("d_di", [2048, 2]),
                       ("d_gt", [2048, 2])]:
            dbgd[nm] = nc.dram_tensor(nm, sh, F32, kind="ExternalOutput").ap()

    es = ExitStack()
    with es:
        def sb(name, shape, dt=F32):
            return es.enter_context(nc.sbuf_tensor(name, shape, dt))

        def psum(name, shape, dt=F32):
            return es.enter_context(nc.psum_tensor(name, shape, dt))

        BR = {}

        def breg(e):
            if 'r' not in BR:
                BR['r'] = e.to_reg(NE * CAP + 127)
            return BR['r']

        def DMA(eng, out, in_, r, w, key, nosync=False):
            S.add(eng, lambda e: e.dma_start(out=out, in_=in_), reads=r, writes=w, dma=key, nosync=nosync)

        def MM(out, lhsT, rhs, st, sp_, r, w):
            S.add("pe", lambda e: e.matmul(out, lhsT=lhsT, rhs=rhs, start=st, stop=sp_), reads=r, writes=w)

        def TR(out, in_, ident, r, w):
            S.add("pe", lambda e: e.transpose(out=out, in_=in_, identity=ident), reads=r, writes=w)

        def ACT(out, in_, func, r, w, bias=None, scale=None, accum=None):
            kw = {}
            if bias is not None:
                kw["bias"] = bias
            if scale is not None:
                kw["scale"] = scale
            if accum is not None:
                kw["accum_out"] = accum
            S.add("act", lambda e: e.activation(out=out, in_=in_, func=func, **kw), reads=r, writes=w)

        def TS(eng, out, in0, s1, s2, op0, op1, r, w):
            if op1 is None:
                S.add(eng, lambda e: e.tensor_scalar(out=out, in0=in0, scalar1=s1, scalar2=None, op0=op0), reads=r, writes=w)
            else:
                S.add(eng, lambda e: e.tensor_scalar(out=out, in0=in0, scalar1=s1, scalar2=s2, op0=op0, op1=op1), reads=r, writes=w)

        def STT(out, in0, scalar, in1, op0, op1, r, w, accum=None):
            if accum is None:
                S.add("dve", lambda e: e.scalar_tensor_tensor(out=out, in0=in0, scalar=scalar, in1=in1, op0=op0, op1=op1), reads=r, writes=w)
            else:
                S.add("dve", lambda e: e.scalar_tensor_tensor(out=out, in0=in0, scalar=scalar, in1=in1, op0=op0, op1=op1, accum_out=accum), reads=r, writes=w)

        def TT(eng, out, in0, in1, op, r, w):
            S.add(eng, lambda e: e.tensor_tensor(out=out, in0=in0, in1=in1, op=op), reads=r, writes=w)

        def CP(eng, out, in_, r, w):
            S.add(eng, lambda e: e.tensor_copy(out=out, in_=in_), reads=r, writes=w)

        def MSET(eng, ap, val, w):
            S.add(eng, lambda e: e.memset(ap, val), writes=w)

        def RECIP(out, in_, r, w):
            S.add("dve", lambda e: e.reciprocal(out=out, in_=in_), reads=r, writes=w)

        def SCAN(out, d0, d1, init, op0, op1, r, w):
            S.add("dve", lambda e: e.tensor_tensor_scan(out=out, data0=d0, data1=d1, initial=init, op0=op0, op1=op1), reads=r, writes=w)

        ARN = 41024
        arena = sb("arena", [128, ARN], BF16)
        win = arena[:, 0:22592].rearrange("p (r c) -> p r c", r=8)
        wout = arena[:, 22592:30784].rearrange("p (r c) -> p r c", r=8)
        wckv = wout
        wcq = arena[:, 30784:34880].rearrange("p (r c) -> p r c", r=8)
        wco = arena[:, 34880:38976].rearrange("p (r c) -> p r c", r=4)
        wkd = arena[:, 38976:41024].rearrange("p (r c) -> p r c", r=8)
        bc = sb("bc", [128, 4 * 1024 + 128])
        G_MIX, G_CROSS, G_FFN, G_AM, MISC = 0, 1024, 2048, 3072, 4096
        pp = sb("pp", [128, 128])
        cmf = sb("cmf", [128, 5, 128])
        identf = cmf[:, 0, :]
        cmb = sb("cmb", [128, 5, 128], BF16)
        identb = cmb[:, 0, :]
        wr = sb("wr", [128, 8, 36], BF16)
        KcT = sb("KcT", [128, 4, 256], BF16)
        Vc = sb("Vc", [128, 2, 4, 129], BF16)
        CN = sb("CN", [128, 4, 129])
        CNb = sb("CNb", [128, 4, 129], BF16)
        stg = sb("stg", [128, 8, 3 + GT])
        gt = sb("gt", [128, NT, 2])
        di = sb("di", [128, NT, 2], I32)
        Acum = sb("Acum", [128, NE], BF16)
        sinkt = sb("sinkt", [128, 8])
        ones_b = sb("ones_b", [128, 128], BF16)
        ones4 = sb("ones4", [4, 128])
        qaT = [sb("qaT%d" % i, [128, 4, GT], BF16) for i in range(2)]
        kaT = [sb("kaT%d" % i, [128, 2, 128 + GT], BF16) for i in range(2)]
        va = [sb("va%d" % i, [128, 3, 2, 65], BF16) for i in range(2)]
        qkT = [sb("qkT%d" % i, [128, 8, GT], BF16) for i in range(2)]
        vm = [sb("vm%d" % i, [128, 2, 4, 129], BF16) for i in range(2)]
        so = [sb("so%d" % i, [128, 2, 512], BF16) for i in range(2)]
        gsc = [sb("gsc%d" % i, [128, 2, 2, 4]) for i in range(2)]
        dcy = [sb("dcy%d" % i, [128, 2, 4]) for i in range(2)]
        xt = [sb("xt%d" % i, [128, 1024]) for i in range(2)]
        xr = [sb("xr%d" % i, [128, 1024]) for i in range(2)]
        bfA = [sb("bfA%d" % i, [128, 1024], BF16) for i in range(3)]
        tpA = [sb("tpA%d" % i, [128, 8, 128], BF16) for i in range(2)]
        hT = sb("hT", [128, 8, GT], BF16)
        junk = sb("junk", [128, 1024], BF16)
        cacc = [sb("cacc%d" % i, [128, GT]) for i in range(2)]
        st8 = sb("st8", [128, 32])
        e1 = sb("e1", [4, GT])
        sp1 = sb("sp1", [4, GT])
        Bn = sb("Bn", [4, 1 + GT])
        ug = sb("ug", [4, GT])
        Ub = sb("Ub", [4, 1 + GT])
        BnC = sb("BnC", [4, 1])
        UC = sb("UC", [4, 1])
        nb = sb("nb", [4, 4])
        scT = sb("scT", [4, 2, GT])
        dx = sb("dx", [4, 2, 4])
        zer4 = sb("zer4", [4, GT])
        one4 = sb("one4", [4, GT])
        nbf = sb("nbf", [4, 1])
        PT = [sb("PT%d" % i, [128, 512], BF16) for i in range(2)]
        smT = [sb("smT%d" % i, [128, 128], BF16) for i in range(2)]
        kp = [sb("kp%d" % i, [128, 128], BF16) for i in range(2)]
        att_o = sb("att_o", [128, 512])
        hmix = sb("hmix", [128, 512])
        qcT = sb("qcT", [128, 4, 128], BF16)
        ocp = sb("ocp", [128, 512], BF16)
        ocT = sb("ocT", [128, 4, 128], BF16)
        lg = sb("lg", [128, 36])
        rt = sb("rt", [128, 96])
        A1 = sb("A1", [128, NE])
        A2 = sb("A2", [128, NE])
        Ab = sb("Ab", [128, NE], BF16)
        cbuf = sb("cbuf", [128, NE])
        ig0 = sb("ig0", [128, 1], I32)
        ocpB = sb("ocpB", [128, 512], BF16)
        ocTB = sb("ocTB", [128, 4, 128], BF16)
        mixb = [sb("mixb%d" % i, [128, 1024], BF16) for i in range(2)]
        ybuf = sb("ybuf", [128, 1024], BF16)
        PcT = [sb("PcT%d" % i, [128, 512], BF16) for i in range(2)]
        ig1 = sb("ig1", [128, 1], I32)
        ig2 = sb("ig2", [128, 1], I32)
        ig3 = sb("ig3", [128, 1], I32)

        T0 = psum("T0", [128, 8, 128], BF16)
        T1 = psum("T1", [128, 8, 128], BF16)
        PA = psum("PA", [128, 512])
        PB = psum("PB", [128, 512])
        PS_ = psum("PS_", [128, 512])
        PV = psum("PV", [128, 512])
        PF = psum("PF", [128, 512])
        PG = psum("PG", [128, 512])
        projbanks = [(PA, "PA"), (PB, "PB")]
        pbi = [0]

        def nextbank():
            b = projbanks[pbi[0] % 2]
            pbi[0] += 1
            return b

        DMA("sp", pp[:], ppd, [], ["pp"], "pp")
        DMA("sp", cmf[:], cmd, [], ["cmf"], "cmf")
        DMA("sp", bc[:, 0:4096], bcd[:, 0:4096], [], ["g_mix", "g_cross", "g_ffn", "g_am"], "bc0")
        DMA("sp", bc[:, 4096:4224], bcd[:, 6144:6272], [], ["misc"], "bc1")
        DMA("sp", xr[1][:], bcd[:, 5120:6144], [], ["xr1"], "xr1")
        CP("dve", cmb[:], cmf[:], ["cmf"], ["cmb"])
        MSET("pool", ones_b[:], 1.0, ["ones_b"])
        MSET("pool", hT[:, 0:4, :], 0.0, ["hT"])
        NZB = (NE * CAP + 128) // 128
        xs_v = xs_d.rearrange("(n p) c -> p n c", p=128)
        for z0 in range(0, NZB, 13):
            z1 = min(NZB, z0 + 13)
            DMA("sp", xs_v[:, z0:z1, :], hT[:, 0:4, :].rearrange("p r c -> p (r c)").unsqueeze(1).to_broadcast([128, z1 - z0, 1024]), ["hT"], ["xs_d"], "xsz", nosync=True)
        print("sbuf remaining", nc.sbuf_bytes_remaining)
        MSET("pool", ones4[:], 1.0, ["ones4"])
        MSET("pool", zer4[:], 0.0, ["zer4"])
        MSET("pool", one4[:], 1.0, ["one4"])
        MSET("pool", stg[:], 0.0, ["stg%d" % c for c in range(8)])
        MSET("pool", BnC[:], 0.0, ["BnC"])
        MSET("pool", UC[:], 0.0, ["UC"])
        MSET("pool", Acum[:], 0.0, ["Acum"])
        MSET("pool", CN[:], 0.0, ["CN%d" % h for h in range(4)])
        for i in range(2):
            MSET("pool", va[i][:, :, :, 64:65], 1.0, ["va%d" % i])
            MSET("pool", vm[i][:, :, :, 128:129], 1.0, ["vm%d" % i])
        MSET("pool", Vc[:, :, :, 128:129], 1.0, ["Vc"])
        PP_CW, PP_CB, PP_BI, PP_BF, PP_FLAG, PP_PM, PP_AB, PP_CQ, PP_BASE = 0, 32, 40, 41, 42, 43, 44, 60, 68
        cw = pp[:, PP_CW:PP_CW + 32].rearrange("p (c j) -> p c j", j=4)
        TS("dve", nbf[:], pp[0:4, PP_BF:PP_BF + 1], -1.0, None, ALU.mult, None, ["pp"], ["nbf"])
        TT("dve", sinkt[:], pp[:, PP_CQ:PP_CQ + 8], bc[:, MISC + 36:MISC + 44], ALU.add, ["pp", "misc"], ["sinkt"])
        ACT(sinkt[:], sinkt[:], AF.Exp, ["sinkt"], ["sinkt"])

        def wload(dst2d, src2d, ncols, key, rkeys):
            c0 = 0
            i = 0
            while c0 < ncols:
                c1 = min(ncols, c0 + 2048)
                DMA("pool", dst2d[:, c0:c1], src2d[:, c0:c1], [], [rkeys[i]], key, nosync=True)
                c0 = c1
                i += 1

        WIN_K = ["win%d" % i for i in range(12)]
        WOUT_K = ["wout%d" % i for i in range(4)]
        WCQ_K = ["wcq%d" % i for i in range(2)]
        WCO_K = ["wco%d" % i for i in range(2)]
        wload(arena[:, 22592:30784], w_ckv.rearrange("(p r) c -> p (r c)", r=8), 8192, "wckv", WOUT_K)
        wload(arena[:, 0:22592], w_in.rearrange("(p r) c -> p (r c)", r=8), 22592, "win", WIN_K)
        S.add("pool", lambda e: e.dma_start(out=wr[:].rearrange("p r c -> p (r c)"), in_=w_r.rearrange("(p r) c -> p (r c)", r=8)), writes=["wr"], dma="wr")

        def rstd_from_ss(col, nfeat, r, w):
            ACT(st8[:, col:col + 1], st8[:, col:col + 1], AF.Ln, r, w, bias=EPS, scale=1.0 / nfeat)
            ACT(st8[:, col:col + 1], st8[:, col:col + 1], AF.Exp, w, w, scale=-0.5)

        def norm_to_bf(src, srckeys, gain, gkey, dst, dstkey, R=8):
            ACT(junk[:], src, AF.Square, srckeys, ["ss"], accum=st8[:, 0:1])
            rstd_from_ss(0, 1024, ["ss"], ["ss"])
            STT(dst, src, st8[:, 0:1], gain, ALU.mult, ALU.mult, srckeys + ["ss", gkey], [dstkey])

        def transpose8(src_bf, srckey, dstT, dstkey, nchunk=8, bank=None, bankkey=None):
            bank = T0 if bank is None else bank
            bankkey = "T0" if bankkey is None else bankkey
            for r in range(nchunk):
                TR(bank[:, r, :], src_bf[:, r:128 * nchunk:nchunk], identb, [srckey, "cmb"], [bankkey])
            S.add("act", lambda e: e.copy(out=dstT, in_=bank[:, 0:nchunk, :]), reads=[bankkey], writes=[dstkey])

        for mt in range(2):
            DMA("sp", xt[mt][:], memb[mt * 128:(mt + 1) * 128, :], [], ["xt%d" % mt], "xt%d" % mt)
            norm_to_bf(xt[mt][:], ["xt%d" % mt], xr[1][:], "xr1", bfA[mt][:], "bfA%d" % mt)
            transpose8(bfA[mt][:], "bfA%d" % mt, tpA[mt][:], "tpA%d" % mt)
        for h in range(4):
            bk, bkk = nextbank()
            for mt in range(2):
                for r in range(8):
                    MM(bk[:, mt * 128:(mt + 1) * 128], wckv[:, r, h * 128:(h + 1) * 128], tpA[mt][:, r, :], r == 0, r == 7,
                       WOUT_K + ["tpA%d" % mt], [bkk])
            CP("dve", KcT[:, h, :], bk[:, 0:256], [bkk], ["KcT"])
        for mt in range(2):
            bk, bkk = nextbank()
            for r in range(8):
                MM(bk[:, :], tpA[mt][:, r, :], wckv[:, r, 512:1024], r == 0, r == 7, WOUT_K + ["tpA%d" % mt], [bkk])
            CP("dve", Vc[:, mt, :, 0:128], bk[:, :].rearrange("p (h d) -> p h d", h=4), [bkk], ["Vc"])
        wload(arena[:, 22592:30784], w_out.rearrange("(p r) c -> p (r c)", r=8), 8192, "wout", WOUT_K)
        wload(arena[:, 30784:34880], w_cq.rearrange("(p r) c -> p (r c)", r=8), 4096, "wcq", WCQ_K)
        wload(arena[:, 34880:38976], w_co.rearrange("(p r) c -> p (r c)", r=4), 4096, "wco", WCO_K)
        for j in range(4):
            kv = j // 2
            CP("pool", wkd[:, :, j * 64:(j + 1) * 64], win[:, :, 512 + kv * 64:512 + (kv + 1) * 64], WIN_K, ["wkd"])

        def load_norm_transpose2(src, t0):
            T1K = ["T1_0", "T1_1", "T1_2", "T1_3", "T1o"]
            xb = [xt[(t0 + j) % 2] for j in range(2)]
            xk = ["xt%d" % ((t0 + j) % 2) for j in range(2)]
            bf = [bfA[(t0 + j) % 3] for j in range(2)]
            bfk = ["bfA%d" % ((t0 + j) % 3) for j in range(2)]
            col = [0, 16]
            ssk = ["ss", "ss2"]
            for j in range(2):
                DMA("sp", xb[j][:], src[(t0 + j) * 128:(t0 + j + 1) * 128, :], [], [xk[j]], xk[j])
            for j in range(2):
                ACT(junk[:], xb[j][:], AF.Square, [xk[j]], [ssk[j]], accum=st8[:, col[j]:col[j] + 1])
                ACT(st8[:, col[j]:col[j] + 1], st8[:, col[j]:col[j] + 1], AF.Ln, [ssk[j]], [ssk[j]], bias=EPS, scale=1.0 / 1024)
                ACT(st8[:, col[j]:col[j] + 1], st8[:, col[j]:col[j] + 1], AF.Exp, [ssk[j]], [ssk[j]], scale=-0.5)
            for j in range(2):
                STT(bf[j][:], xb[j][:], st8[:, col[j]:col[j] + 1], bc[:, G_MIX:G_MIX + 1024], ALU.mult, ALU.mult, [xk[j], ssk[j], "g_mix"], [bfk[j]])
            for r in range(8):
                TR(T0[:, r, :], bf[0][:, r:1024:8], identb, [bfk[0], "cmb"], ["T0"])
            for r in range(8):
                TR(T1[:, r, :], bf[1][:, r:1024:8], identb, [bfk[1], "cmb"], T1K)
            S.add("act", lambda e: e.copy(out=hT[:, :, 0:128], in_=T0[:, :, :]), reads=["T0"], writes=["hT"])
            S.add("act", lambda e: e.copy(out=hT[:, :, 128:256], in_=T1[:, :, :]), reads=T1K, writes=["hT"])

        def fm_proj(wsel, wkeys, evac):
            bk, bkk = nextbank()
            for r in range(8):
                MM(bk[:, 0:GT], wsel(r), hT[:, r, :], r == 0, r == 7, wkeys + ["hT"], [bkk])
            evac(bk, bkk)

        def conv_chunk(c, gp):
            def ev(bk, bkk):
                sk = "stg%d" % c
                CP("pool", stg[:, c, 0:3], stg[:, c, GT:GT + 3], [sk], [sk])
                S.add("act", lambda e: e.copy(out=stg[:, c, 3:3 + GT], in_=bk[:, 0:GT]), reads=[bkk], writes=[sk])
                flush_silu()
                ca = cacc[c % 2]
                cak = "cacc%d" % (c % 2)
                TS("dve", ca[:], stg[:, c, 0:GT], cw[:, c, 0:1], None, ALU.mult, None, [sk, "pp"], [cak])
                for jj in range(1, 4):
                    STT(ca[:], stg[:, c, jj:jj + GT], cw[:, c, jj:jj + 1], ca[:], ALU.mult, ALU.add, [sk, "pp", cak], [cak])
                PEND.append(lambda: ACT(qkT[gp][:, c, :], ca[:], AF.Silu, [cak, "pp"], ["qkT%d_%d" % (gp, c)], bias=pp[:, PP_CB + c:PP_CB + c + 1]))
            fm_proj(lambda r: win[:, r, 768 + c * 128:768 + (c + 1) * 128], WIN_K, ev)

        PEND = []

        def flush_silu():
            while PEND:
                PEND.pop(0)()

        def gates_group(gp, prefix, first):
            bk, bkk = nextbank()
            for r in range(8):
                MM(bk[0:4, 0:GT], win[:, r, 2816:2820], hT[:, r, :], r == 0, r == 7, WIN_K + ["hT"], [bkk])
            TS("dve", ug[:], bk[0:4, 0:GT], pp[0:4, PP_BI:PP_BI + 1], None, ALU.add, None, [bkk, "pp"], ["ug"])
            bk2, bkk2 = nextbank()
            for r in range(8):
                MM(bk2[0:4, 0:GT], win[:, r, 2820:2824], hT[:, r, :], r == 0, r == 7, WIN_K + ["hT"], [bkk2])
            ACT(e1[:], bk2[0:4, 0:GT], AF.Exp, [bkk2, "nbf"], ["e1"], bias=nbf[:], scale=-1.0)
            yield
            ACT(sp1[:], e1[:], AF.Ln, ["e1"], ["sp1"], bias=1.0)
            yield
            if prefix:
                TS("dve", sp1[:], sp1[:], pp[0:4, PP_FLAG:PP_FLAG + 1], None, ALU.mult, None, ["sp1", "pp"], ["sp1"])
            SCAN(Bn[:, 1:1 + GT], one4[:], sp1[:], BnC[:], ALU.mult, ALU.add, ["one4", "sp1", "BnC", "Bn"], ["Bn"])
            yield
            CP("dve", BnC[:], Bn[:, GT:GT + 1], ["Bn"], ["BnC"])
            TT("dve", ug[:], ug[:], Bn[:, 1:1 + GT], ALU.add, ["ug", "Bn"], ["ug"])
            yield
            if prefix:
                TS("dve", ug[:], ug[:], pp[0:4, PP_FLAG:PP_FLAG + 1], pp[0:4, PP_PM:PP_PM + 1], ALU.mult, ALU.add, ["ug", "pp"], ["ug"])
            CP("dve", Ub[:, 0:1], UC[:], ["UC"], ["Ub"])
            yield
            SCAN(Ub[:, 1:1 + GT], ug[:], zer4[:], UC[:], ALU.max, ALU.max, ["ug", "zer4", "UC", "Ub"], ["Ub"])
            yield
            CP("dve", UC[:], Ub[:, GT:GT + 1], ["Ub"], ["UC"])
            TS("dve", nb[:, 0:2], Ub[:, 0:GT:128], -1.0, -LN_SQRT_DH, ALU.mult, ALU.add, ["Ub"], ["nb"])
            TS("dve", nb[:, 2:4], Ub[:, 0:GT:128], -1.0, None, ALU.mult, None, ["Ub"], ["nb"])
            yield
            for cc in range(2):
                ACT(scT[:, 0, cc * 128:(cc + 1) * 128], ug[:, cc * 128:(cc + 1) * 128], AF.Exp, ["ug", "nb"], ["scT"], bias=nb[:, cc:cc + 1])
                if not prefix:
                    ACT(scT[:, 1, cc * 128:(cc + 1) * 128], Bn[:, 1 + cc * 128:1 + (cc + 1) * 128], AF.Exp, ["Bn", "nb"], ["scT"], bias=nb[:, 2 + cc:3 + cc])
                    yield
            TT("dve", nb[:, 0:2], Ub[:, 0:GT:128], Ub[:, 128:GT + 1:128], ALU.subtract, ["Ub", "nb", "scT"], ["nb"])
            yield
            ACT(nb[:, 0:2], nb[:, 0:2], AF.Exp, ["nb"], ["nb"])
            yield
            TT("dve", dx[:], nb[:, 0:2].unsqueeze(2).to_broadcast([4, 2, 4]), identf[0:4, 0:4].unsqueeze(1).to_broadcast([4, 2, 4]), ALU.mult, ["nb", "cmf"], ["dx"])
            yield
            bk3, bkk3 = nextbank()
            MM(bk3[:, 0:8], ones4[:, :], dx[:].rearrange("p c h -> p (c h)"), True, True, ["ones4", "dx"], [bkk3])
            CP("dve", dcy[gp][:].rearrange("p c h -> p (c h)"), bk3[:, 0:8], [bkk3], ["dcy%d" % gp])
            bk4, bkk4 = nextbank()
            nwh = 1 if prefix else 2
            for cc in range(2):
                for wh in range(nwh):
                    TR(bk4[:, (cc * 2 + wh) * 4:(cc * 2 + wh) * 4 + 4], scT[:, wh, cc * 128:(cc + 1) * 128], identf[0:4, 0:4], ["scT", "cmf"], [bkk4])
            if prefix:
                for cc in range(2):
                    CP("dve", gsc[gp][:, cc, 0, :], bk4[:, cc * 8:cc * 8 + 4], [bkk4], ["gsc%d" % gp])
            else:
                CP("dve", gsc[gp][:].rearrange("p c w h -> p (c w h)"), bk4[:, 0:16], [bkk4], ["gsc%d" % gp])

        def adv(gen, k):
            for _ in range(k):
                next(gen, None)

        def drain(gen):
            for _ in gen:
                pass

        def state_update(gp, j, h, first):
            kT = qkT[gp][:, 4 + h, j * 128:(j + 1) * 128]
            TR(T1[:, h, :], kT, identb, ["qkT%d_%d" % (gp, 4 + h), "cmb"], ["T1_%d" % h])
            kpb = kp[h % 2]
            kpk = "kp%d" % (h % 2)
            TS("dve", kpb[:], T1[:, h, :], gsc[gp][:, j, 0, h:h + 1], None, ALU.mult, None, ["T1_%d" % h, "gsc%d" % gp], [kpk])
            pf = PF if h % 2 == 0 else PG
            pfk = "PF" if h % 2 == 0 else "PG"
            MM(pf[:, 257:386], kpb[:], vm[gp][:, j, h, :], True, True, [kpk, "vm%d" % gp], [pfk])
            ck = "CN%d" % h
            if first:
                CP("dve", CN[:, h, :], pf[:, 257:386], [pfk], [ck])
            else:
                STT(CN[:, h, :], CN[:, h, :], st8[:, 4 + h:5 + h], pf[:, 257:386], ALU.mult, ALU.add, [pfk, ck, "dprev%d" % h], [ck])
            TS("dve", CNb[:, h, :], CN[:, h, :], dcy[gp][:, j, h:h + 1], None, ALU.mult, None, [ck, "dcy%d" % gp], ["CNb%d" % h])
            CP("dve", st8[:, 4 + h:5 + h], dcy[gp][:, j, h:h + 1], ["dcy%d" % gp, ck], ["dprev%d" % h])

        def prefix_proj(pg):
            gp = pg % 2
            load_norm_transpose2(xp, pg * 2)
            gg = gates_group(gp, True, pg == 0)
            adv(gg, 1)
            crange = range(8) if pg == NG - 1 else range(4, 8)
            for c in crange:
                conv_chunk(c, gp)
                adv(gg, 2)
            flush_silu()
            for j in range(2):
                bk, bkk = nextbank()
                for r in range(8):
                    MM(bk[:, :], hT[:, r, j * 128:(j + 1) * 128], win[:, r, 1792:2304], r == 0, r == 7, WIN_K + ["hT"], [bkk])
                CP("dve", vm[gp][:, j, :, 0:128], bk[:, :].rearrange("p (h d) -> p h d", h=4), [bkk], ["vm%d" % gp])
                adv(gg, 2)
            if pg == NG - 1:
                for kv in range(2):
                    def ev(bk, bkk, kv=kv):
                        S.add("act", lambda e: e.copy(out=kaT[gp][:, kv, 128:128 + GT], in_=bk[:, 0:GT]), reads=[bkk], writes=["kaT%d" % gp])
                    fm_proj(lambda r, kv=kv: wkd[:, r, kv * 128:(kv + 1) * 128], ["wkd"], ev)
                bk, bkk = nextbank()
                for r in range(8):
                    MM(bk[:, 0:128], hT[:, r, 128:256], win[:, r, 640:768], r == 0, r == 7, WIN_K + ["hT"], [bkk])
                CP("dve", va[gp][:, 2, :, 0:64], bk[:, 0:128].rearrange("p (k d) -> p k d", k=2), [bkk], ["va%d" % gp])
            return gg

        def prefix_state(pg):
            gp = pg % 2
            for j in range(2):
                for h in range(4):
                    state_update(gp, j, h, pg == 0 and j == 0)

        NPG = NG if stage >= 2 else 0
        for pg in range(NPG):
            gg_ = prefix_proj(pg)
            if pg >= 1:
                prefix_state(pg - 1)
            drain(gg_)

        def own_proj(g):
            gp = g % 2
            load_norm_transpose2(xo, g * 2)
            gg = gates_group(gp, False, False)
            adv(gg, 1)
            for c in range(4):
                def ev(bk, bkk, c=c):
                    S.add("act", lambda e: e.copy(out=qaT[gp][:, c, :], in_=bk[:, 0:GT]), reads=[bkk], writes=["qaT%d" % gp])
                fm_proj(lambda r, c=c: win[:, r, c * 128:(c + 1) * 128], WIN_K, ev)
            CP("pool", kaT[gp][:, :, 0:128], kaT[1 - gp][:, :, GT:GT + 128], ["kaT%d" % (1 - gp)], ["kaT%d" % gp])
            for kv in range(2):
                def ev(bk, bkk, kv=kv):
                    S.add("act", lambda e: e.copy(out=kaT[gp][:, kv, 128:128 + GT], in_=bk[:, 0:GT]), reads=[bkk], writes=["kaT%d" % gp])
                fm_proj(lambda r, kv=kv: wkd[:, r, kv * 128:(kv + 1) * 128], ["wkd"], ev)
            for c in range(8):
                conv_chunk(c, gp)
                adv(gg, 1)
            flush_silu()
            CP("pool", va[gp][:, 0, :, 0:64], va[1 - gp][:, 2, :, 0:64], ["va%d" % (1 - gp)], ["va%d" % gp])
            for j in range(2):
                bk, bkk = nextbank()
                for r in range(8):
                    MM(bk[:, 0:128], hT[:, r, j * 128:(j + 1) * 128], win[:, r, 640:768], r == 0, r == 7, WIN_K + ["hT"], [bkk])
                CP("dve", va[gp][:, 1 + j, :, 0:64], bk[:, 0:128].rearrange("p (k d) -> p k d", k=2), [bkk], ["va%d" % gp])
                bk, bkk = nextbank()
                for r in range(8):
                    MM(bk[:, :], hT[:, r, j * 128:(j + 1) * 128], win[:, r, 1792:2304], r == 0, r == 7, WIN_K + ["hT"], [bkk])
                CP("dve", vm[gp][:, j, :, 0:128], bk[:, :].rearrange("p (h d) -> p h d", h=4), [bkk], ["vm%d" % gp])
                bk, bkk = nextbank()
                for r in range(8):
                    MM(bk[:, :], hT[:, r, j * 128:(j + 1) * 128], win[:, r, 2304:2816], r == 0, r == 7, WIN_K + ["hT"], [bkk])
                ACT(so[gp][:, j, :], bk[:, :], AF.Sigmoid, [bkk], ["so%d" % gp])
                adv(gg, 2)
            return gg

        def mixX(g, j):
            gp = g % 2
            ti = g * 2 + j
            for kvg in range(2):
                for kb in range(2):
                    for hh in range(4):
                        hd = kvg * 4 + hh
                        ch, po = hd // 2, (hd % 2) * 64
                        sbk, sbkk = (PS_, "PS") if hh % 2 == 0 else (PV, "PV")
                        MM(sbk[:, (hh // 2) * 128:(hh // 2 + 1) * 128], kaT[gp][po:po + 64, kvg, (j + kb) * 128:(j + kb + 1) * 128],
                           qaT[gp][po:po + 64, ch, j * 128:(j + 1) * 128], True, True, ["kaT%d" % gp, "qaT%d" % gp], [sbkk])
                    for hh in range(4):
                        hd = kvg * 4 + hh
                        sbk, sbkk = (PS_, "PS") if hh % 2 == 0 else (PV, "PV")
                        ACT(PT[kb][:, hh * 128:(hh + 1) * 128], sbk[:, (hh // 2) * 128:(hh // 2 + 1) * 128], AF.Exp, [sbkk, "pp"], ["PT%d" % kb],
                            bias=pp[:, PP_AB + kb * 8 + hd:PP_AB + kb * 8 + hd + 1], scale=0.125)
                        yield
                    if kb == 0:
                        mk = cmb[:, 3, :] if ti == 0 else cmb[:, 1, :]
                    else:
                        mk = cmb[:, 2, :]
                    TT("dve", PT[kb][:].rearrange("p (h t) -> p h t", h=4), PT[kb][:].rearrange("p (h t) -> p h t", h=4),
                       mk.unsqueeze(1).to_broadcast([128, 4, 128]), ALU.mult, ["PT%d" % kb, "cmb"], ["PT%d" % kb])
                    yield
                for hh in range(4):
                    for kb in range(2):
                        MM(PV[:, hh * 65:(hh + 1) * 65], PT[kb][:, hh * 128:(hh + 1) * 128], va[gp][:, j + kb, kvg, :], kb == 0, kb == 1,
                           ["PT%d" % kb, "va%d" % gp], ["PV"])
                        yield
                pv3 = PV[:, 0:260].rearrange("p (h d) -> p h d", h=4)
                TT("dve", st8[:, 8:12], pv3[:, :, 64], sinkt[:, kvg * 4:kvg * 4 + 4], ALU.add, ["PV", "sinkt"], ["dn4"])
                yield
                RECIP(st8[:, 8:12], st8[:, 8:12], ["dn4"], ["dn4"])
                yield
                TT("dve", att_o[:, kvg * 256:(kvg + 1) * 256].rearrange("p (h d) -> p h d", h=4), pv3[:, :, 0:64],
                   st8[:, 8:12].unsqueeze(2).to_broadcast([128, 4, 64]), ALU.mult, ["PV", "dn4"], ["att_o"])
                yield
            mixp = mixb[ti % 2]
            mixk = "mix%d" % (ti % 2)
            ACT(junk[:, 0:512], att_o[:], AF.Square, ["att_o"], ["ssa"], accum=st8[:, 3:4])
            ACT(st8[:, 3:4], st8[:, 3:4], AF.Ln, ["ssa"], ["ssa"], bias=EPS, scale=1.0 / 512)
            ACT(st8[:, 3:4], st8[:, 3:4], AF.Exp, ["ssa"], ["ssa"], scale=-0.5)
            yield
            STT(mixp[:, 0:512], att_o[:], st8[:, 3:4], bc[:, G_AM:G_AM + 512], ALU.mult, ALU.mult, ["att_o", "ssa", "g_am"], [mixk])
            yield
            if dbg:
                DMA("sp", dbgd["d_att"][ti * 128:(ti + 1) * 128, :], att_o[:], ["att_o"], ["d_att"], "dbg")
            for h in range(4):
                pf = PF if h % 2 == 0 else PG
                pfk = "PF" if h % 2 == 0 else "PG"
                qT = qkT[gp][:, h, j * 128:(j + 1) * 128]
                kT = qkT[gp][:, 4 + h, j * 128:(j + 1) * 128]
                MM(pf[:, 0:128], kT, qT, True, True, ["qkT%d_%d" % (gp, 4 + h), "qkT%d_%d" % (gp, h)], [pfk])
                sm = smT[h % 2]
                smk = "smT%d" % (h % 2)
                STT(sm[:], pf[:, 0:128], gsc[gp][:, j, 0, h:h + 1], cmf[:, 2, :], ALU.mult, ALU.mult, [pfk, "gsc%d" % gp, "cmf"], [smk])
                yield
                MM(pf[:, 128:257], sm[:], vm[gp][:, j, h, :], True, False, [smk, "vm%d" % gp], [pfk])
                MM(pf[:, 128:257], qT, CNb[:, h, :], False, True, ["qkT%d_%d" % (gp, h), "CNb%d" % h], [pfk])
                yield
                TS("dve", st8[:, 1:2], pf[:, 256:257], -1.0, None, ALU.mult, None, [pfk], ["dn1"])
                TT("dve", st8[:, 1:2], st8[:, 1:2], pf[:, 256:257], ALU.max, [pfk, "dn1"], ["dn1"])
                yield
                TS("dve", st8[:, 1:2], st8[:, 1:2], gsc[gp][:, j, 1, h:h + 1], None, ALU.max, None, ["dn1", "gsc%d" % gp], ["dn1"])
                RECIP(st8[:, 1:2], st8[:, 1:2], ["dn1"], ["dn1"])
                yield
                STT(hmix[:, h * 128:(h + 1) * 128], pf[:, 128:256], st8[:, 1:2], so[gp][:, j, h * 128:(h + 1) * 128], ALU.mult, ALU.mult,
                    [pfk, "dn1", "so%d" % gp], ["hmix%d" % h])
                yield
                state_update(gp, j, h, False)
                yield
                ACT(junk[:, 0:128], hmix[:, h * 128:(h + 1) * 128], AF.Square, ["hmix%d" % h], ["ssh"], accum=st8[:, 2:3])
                ACT(st8[:, 2:3], st8[:, 2:3], AF.Ln, ["ssh"], ["ssh"], bias=EPS, scale=1.0 / 128)
                ACT(st8[:, 2:3], st8[:, 2:3], AF.Exp, ["ssh"], ["ssh"], scale=-0.5)
                yield
                STT(mixp[:, 512 + h * 128:512 + (h + 1) * 128],
                    hmix[:, h * 128:(h + 1) * 128], st8[:, 2:3],
                    bc[:, G_AM + 512 + h * 128:G_AM + 512 + (h + 1) * 128], ALU.mult, ALU.mult,
                    ["hmix%d" % h, "ssh", "g_am"], [mixk])
                yield
            if dbg:
                DMA("sp", dbgd["d_hm"][ti * 128:(ti + 1) * 128, :], hmix[:], ["hmix%d" % h for h in range(4)], ["d_hm"], "dbg")
                yield

        def mixY(g, j):
            gp = g % 2
            ti = g * 2 + j
            xrb = xr[ti % 2]
            xrk = "xr%d" % (ti % 2)
            if ti == 0:
                DMA("sp", xrb[:], xo[0:128, :], [], [xrk], xrk)
            if ti + 1 < NT:
                DMA("sp", xr[(ti + 1) % 2][:], xo[(ti + 1) * 128:(ti + 2) * 128, :], [], ["xr%d" % ((ti + 1) % 2)], "xr%d" % ((ti + 1) % 2))
            mT = tpA[ti % 2]
            mTk = "tpA%d" % (ti % 2)
            mixp = mixb[ti % 2]
            mixk = "mix%d" % (ti % 2)
            transpose8(mixp[:], mixk, mT[:], mTk)
            yield
            for nh in range(2):
                bk, bkk = nextbank()
                for r in range(8):
                    MM(bk[:, :], mT[:, r, :], wout[:, r, nh * 512:(nh + 1) * 512], r == 0, r == 7, [mTk] + WOUT_K, [bkk])
                TT("dve", xrb[:, nh * 512:(nh + 1) * 512], bk[:, :], xrb[:, nh * 512:(nh + 1) * 512], ALU.add, [bkk, xrk], [xrk])
                yield
            if dbg:
                DMA("sp", dbgd["d_x1"][ti * 128:(ti + 1) * 128, :], xrb[:], [xrk], ["d_x1"], "dbg")
            xcp = ybuf
            xck = "ybuf"
            norm_to_bf(xrb[:], [xrk], bc[:, G_CROSS:G_CROSS + 1024], "g_cross", xcp[:], xck)
            yield
            xcT = tpA[(ti + 1) % 2]
            xcTk = "tpA%d" % ((ti + 1) % 2)
            transpose8(xcp[:], xck, xcT[:], xcTk)
            yield
            bk, bkk = nextbank()
            for h in range(4):
                for r in range(8):
                    MM(bk[:, h * 128:(h + 1) * 128], wcq[:, r, h * 128:(h + 1) * 128], xcT[:, r, :], r == 0, r == 7, WCQ_K + [xcTk], [bkk])
            S.add("act", lambda e, bk=bk: e.copy(out=qcT[:].rearrange("p h t -> p (h t)"), in_=bk[:, :]), reads=[bkk], writes=["qcT"])
            yield
            for mt in range(2):
                pb = PA if mt == 0 else PB
                pbk = "PA" if mt == 0 else "PB"
                for h in range(4):
                    MM(pb[:, h * 128:(h + 1) * 128], KcT[:, h, mt * 128:(mt + 1) * 128], qcT[:, h, :], True, True, ["KcT", "qcT"], [pbk])
                ACT(PcT[mt][:], pb[:, :], AF.Exp, [pbk], ["PcT%d" % mt], scale=1.0 / math.sqrt(128.0))
            for h in range(4):
                pf = PA if h < 2 else PB
                pfk = "PA" if h < 2 else "PB"
                for mt in range(2):
                    MM(pf[:, (h % 2) * 129:(h % 2 + 1) * 129], PcT[mt][:, h * 128:(h + 1) * 128], Vc[:, mt, h, :], mt == 0, mt == 1, ["PcT%d" % mt, "Vc"], [pfk])
            for hp in range(2):
                pf = PA if hp == 0 else PB
                pfk = "PA" if hp == 0 else "PB"
                p3 = pf[:, 0:258].rearrange("p (h d) -> p h d", h=2)
                RECIP(st8[:, 12 + 2 * hp:12 + 2 * hp + 2], p3[:, :, 128], [pfk], ["dnc%d" % hp])
                for q in range(2):
                    h = hp * 2 + q
                    TS("dve", ocp[:, h * 128:(h + 1) * 128], p3[:, q, 0:128],
                       st8[:, 12 + h:13 + h], None, ALU.mult, None, [pfk, "dnc%d" % hp], ["ocp"])
            yield
            for r in range(4):
                TR(T1[:, 4 + r, :], ocp[:, r:512:4], identb, ["ocp", "cmb"], ["T1o"])
            S.add("act", lambda e: e.copy(out=ocT[:], in_=T1[:, 4:8, :]), reads=["T1o"], writes=["ocT"])
            yield
            for nh in range(2):
                bk, bkk = nextbank()
                for r in range(4):
                    MM(bk[:, :], ocT[:, r, :], wco[:, r, nh * 512:(nh + 1) * 512], r == 0, r == 3, ["ocT"] + WCO_K, [bkk])
                TT("dve", xrb[:, nh * 512:(nh + 1) * 512], bk[:, :], xrb[:, nh * 512:(nh + 1) * 512], ALU.add, [bkk, xrk], [xrk])
                yield
            DMA("sp", x2_d[ti * 128:(ti + 1) * 128, :], xrb[:], [xrk], ["x2d%d" % ti], "x2st%d" % (ti % 2))
            if dbg:
                DMA("sp", dbgd["d_x2"][ti * 128:(ti + 1) * 128, :], xrb[:], [xrk], ["d_x2"], "dbg")
            xnp = ybuf
            xnk = "ybuf"
            norm_to_bf(xrb[:], [xrk], bc[:, G_FFN:G_FFN + 1024], "g_ffn", xnp[:], xnk)
            yield
            xnT = tpA[ti % 2]
            xnTk = "tpA%d" % (ti % 2)
            transpose8(xnp[:], xnk, xnT[:], xnTk)
            yield
            bk, bkk = nextbank()
            for r in range(8):
                MM(bk[:, 0:36], xnT[:, r, :], wr[:, r, :], r == 0, r == 7, [xnTk, "wr"], [bkk])
            TT("dve", lg[:], bk[:, 0:36], bc[:, MISC:MISC + 36], ALU.add, [bkk, "misc"], ["lg"])
            yield
            R_GMAX, R_NG, R_GSUM, R_GOH, R_PEN, R_T8, R_D, R_P2, R_D1 = 0, 1, 2, 4, 8, 16, 24, 25, 26
            S.add("dve", lambda e: e.tensor_reduce(out=rt[:, R_GMAX:R_GMAX + 1], in_=lg[:, 0:4], axis=AX.X, op=ALU.max), reads=["lg"], writes=["rt"])
            TS("dve", rt[:, R_NG:R_NG + 1], rt[:, R_GMAX:R_GMAX + 1], -1.0, None, ALU.mult, None, ["rt"], ["rt"])
            yield
            ACT(rt[:, R_GOH:R_GOH + 4], lg[:, 0:4], AF.Exp, ["lg", "rt"], ["rt"], bias=rt[:, R_NG:R_NG + 1], accum=rt[:, R_GSUM:R_GSUM + 1])
            yield
            RECIP(rt[:, R_GSUM:R_GSUM + 1], rt[:, R_GSUM:R_GSUM + 1], ["rt"], ["rt"])
            yield
            TS("dve", rt[:, R_GOH:R_GOH + 4], lg[:, 0:4], rt[:, R_GMAX:R_GMAX + 1], None, ALU.is_equal, None, ["lg", "rt"], ["rt"])
            TS("dve", rt[:, R_PEN:R_PEN + 4], rt[:, R_GOH:R_GOH + 4], 1e30, -1e30, ALU.mult, ALU.add, ["rt"], ["rt"])
            TT("dve", cbuf[:].rearrange("p (g e) -> p g e", g=4), lg[:, 4:36].rearrange("p (g e) -> p g e", g=4),
               rt[:, R_PEN:R_PEN + 4].unsqueeze(2).to_broadcast([128, 4, 8]), ALU.add, ["lg", "rt"], ["cbuf"])
            yield
            S.add("dve", lambda e: e.max(out=rt[:, R_T8:R_T8 + 8], in_=cbuf[:]), reads=["cbuf"], writes=["rt"])
            yield
            TS("dve", A1[:], cbuf[:], rt[:, R_T8:R_T8 + 1], None, ALU.is_equal, None, ["cbuf", "rt"], ["A1"])
            TS("dve", A2[:], cbuf[:], rt[:, R_T8 + 1:R_T8 + 2], None, ALU.is_equal, None, ["cbuf", "rt"], ["A2"])
            yield
            TT("dve", rt[:, R_D:R_D + 1], rt[:, R_T8 + 1:R_T8 + 2], rt[:, R_T8:R_T8 + 1], ALU.subtract, ["rt"], ["rt"])
            ACT(rt[:, R_P2:R_P2 + 1], rt[:, R_D:R_D + 1], AF.Sigmoid, ["rt"], ["rt"])
            yield
            TT("dve", gt[:, ti, 1:2], rt[:, R_P2:R_P2 + 1], rt[:, R_GSUM:R_GSUM + 1], ALU.mult, ["rt"], ["gt"])
            TT("dve", gt[:, ti, 0:1], rt[:, R_GSUM:R_GSUM + 1], gt[:, ti, 1:2], ALU.subtract, ["rt", "gt"], ["gt"])
            TT("dve", Ab[:], A1[:], A2[:], ALU.add, ["A1", "A2"], ["Ab"])
            yield
            bk, bkk = nextbank()
            MM(bk[:, 0:NE], cmb[:, 4, :], Ab[:], True, False, ["cmb", "Ab"], [bkk])
            MM(bk[:, 0:NE], ones_b[:], Acum[:], False, True, ["ones_b", "Acum"], [bkk])
            TT("dve", cbuf[:], bk[:, 0:NE], pp[:, PP_BASE:PP_BASE + NE], ALU.add, [bkk, "pp", "A1", "A2"], ["cbuf"])
            yield
            TT("pool", Acum[:], Acum[:], Ab[:], ALU.add, ["Acum", "Ab"], ["Acum"])
            STT(junk[:, 0:NE], A1[:], 1.0, cbuf[:], ALU.mult, ALU.mult, ["A1", "cbuf"], ["rt"], accum=rt[:, R_D1:R_D1 + 1])
            STT(junk[:, 0:NE], A2[:], 1.0, cbuf[:], ALU.mult, ALU.mult, ["A2", "cbuf"], ["rt"], accum=rt[:, R_D1 + 1:R_D1 + 2])
            yield
            TS("dve", rt[:, R_D1:R_D1 + 2], rt[:, R_D1:R_D1 + 2], float(NE * CAP), None, ALU.min, None, ["rt"], ["rt"])
            CP("dve", di[:, ti, :], rt[:, R_D1:R_D1 + 2], ["rt"], ["di"])
            yield
            for k in range(2 if stage >= 4 else 0):
                S.add("pool", lambda e, k=k, xnp=xnp, ti=ti: e.indirect_dma_start(out=xs_d, out_offset=bass.IndirectOffsetOnAxis(ap=di[:, ti, k:k + 1], axis=0),
                                                                     in_=xnp[:, :], in_offset=None, bounds_check=breg(e), oob_is_err=False),
                      reads=["di", xnk, "xs_d"], writes=["xs_s%d" % k], dma="scat%d" % k)
            if dbg:
                DMA("sp", dbgd["d_lg"][ti * 128:(ti + 1) * 128, :], lg[:], ["lg"], ["d_lg"], "dbg")
                DMA("sp", dbgd["d_gt"][ti * 128:(ti + 1) * 128, :], gt[:, ti, :], ["gt"], ["d_gt"], "dbg")
                DMA("sp", dbgd["d_di"][ti * 128:(ti + 1) * 128, :], rt[:, R_D1:R_D1 + 2], ["rt"], ["d_di"], "dbg")

                yield

        def interleave(gens):
            gens = [g_ for g_ in gens if g_ is not None]
            while gens:
                for g_ in list(gens):
                    try:
                        next(g_)
                    except StopIteration:
                        gens.remove(g_)


        if stage >= 3:
            drain(own_proj(0))
            if NPG > 0:
                prefix_state(NPG - 1)
            gg_ = own_proj(1) if NG > 1 else None
            interleave([mixX(0, 0), gg_])
            for t in range(NT):
                gg_ = None
                if (t + 1) % 2 == 0 and (t + 1) // 2 + 1 < NG:
                    gg_ = own_proj((t + 1) // 2 + 1)
                interleave([mixX((t + 1) // 2, (t + 1) % 2) if t + 1 < NT else None, mixY(t // 2, t % 2), gg_])
        elif NPG > 0:
            prefix_state(NPG - 1)

        PH2_W = WIN_K + WOUT_K + WCQ_K + WCO_K + ["wkd"]
        DMA("sp", bc[:, G_MIX:G_MIX + 1024], bcd[:, 4096:5120], [], ["g_mix"], "bc0")
        NSLOT = 3
        NE_run = int(os.environ.get('MOE_N', NE)) if stage >= 5 else 0
        blocks = [(e_, b) for e_ in range(NE_run) for b in range(CAP // 128)]
        NB = len(blocks)
        ocp2 = [ocp, ocpB]
        ocT2 = [ocT, ocTB]

        def wkeys(e_):
            s_ = e_ % NSLOT
            return ["eg%d" % s_], ["eu%d" % s_], ["ed%d" % s_]

        def wviews(e_):
            base = (e_ % NSLOT) * 12288
            return (arena[:, base:base + 4096].rearrange("p (r c) -> p r c", r=8),
                    arena[:, base + 4096:base + 8192].rearrange("p (r c) -> p r c", r=8),
                    arena[:, base + 8192:base + 12288].rearrange("p (r c) -> p r c", r=4))

        def issue_w(e_):
            s_ = e_ % NSLOT
            base = s_ * 12288
            gk, uk, dk = wkeys(e_)
            extra = PH2_W if e_ < NSLOT else []
            DMA("pool", arena[:, base:base + 4096].rearrange("p (a c) -> p a c", a=2),
                w_eg[e_].rearrange("(p r) c -> p (r c)", r=8).rearrange("p (a c) -> p a c", a=2), [], gk + extra, "eg%d" % s_)
            DMA("pool", arena[:, base + 4096:base + 8192].rearrange("p (a c) -> p a c", a=2),
                w_eu[e_].rearrange("(p r) c -> p (r c)", r=8).rearrange("p (a c) -> p a c", a=2), [], uk + extra, "eu%d" % s_)
            DMA("pool", arena[:, base + 8192:base + 12288].rearrange("p (a c) -> p a c", a=2),
                w_ed[e_].rearrange("(p r) c -> p (r c)", r=4).rearrange("p (a c) -> p a c", a=2), [], dk + extra, "ed%d" % s_)

        def stA(bi):
            e_, b = blocks[bi]
            row0 = e_ * CAP + b * 128
            xg = bfA[bi % 3]
            xgk = "bfA%d" % (bi % 3)
            DMA("sp", xg[:], xs_d[row0:row0 + 128, :], ["xs_d", "xs_s0", "xs_s1"], [xgk], xgk)
            transpose8(xg[:], xgk, tpA[bi % 2][:], "tpA%d" % (bi % 2))

        def stB(bi):
            e_, b = blocks[bi]
            gk, uk, dk = wkeys(e_)
            wg_, wu_, wd_ = wviews(e_)
            xgT = tpA[bi % 2]
            xgTk = "tpA%d" % (bi % 2)
            for r in range(8):
                MM(PA[:, :], xgT[:, r, :], wg_[:, r, :], r == 0, r == 7, [xgTk] + gk, ["PA"])
            for r in range(8):
                MM(PB[:, :], xgT[:, r, :], wu_[:, r, :], r == 0, r == 7, [xgTk] + uk, ["PB"])
            ACT(att_o[:], PA[:, :], AF.Silu, ["PA"], ["att_o"])
            TT("dve", ocp2[bi % 2][:], att_o[:], PB[:, :], ALU.mult,
               ["att_o", "PB"], ["ocp%d" % (bi % 2)])

        def stC1(bi):
            oc = ocp2[bi % 2]
            ot = ocT2[bi % 2]
            for r in range(4):
                TR(T1[:, 4 + r, :], oc[:, r:512:4], identb, ["ocp%d" % (bi % 2), "cmb"], ["T1o"])
            S.add("act", lambda e: e.copy(out=ot[:], in_=T1[:, 4:8, :]), reads=["T1o"], writes=["ocT%d" % (bi % 2)])

        def stC2(bi):
            e_, b = blocks[bi]
            row0 = e_ * CAP + b * 128
            gk, uk, dk = wkeys(e_)
            wg_, wu_, wd_ = wviews(e_)
            ot = ocT2[bi % 2]
            yb = xt[bi % 2]
            ybk = "xt%d" % (bi % 2)
            for nh in range(2):
                pb = PS_ if nh == 0 else PV
                pbk = "PS" if nh == 0 else "PV"
                for r in range(4):
                    MM(pb[:, :], ot[:, r, :], wd_[:, r, nh * 512:(nh + 1) * 512], r == 0, r == 3, ["ocT%d" % (bi % 2)] + dk, [pbk])
                if nh == 0:
                    S.add("act", lambda e, yb=yb, pb=pb: e.copy(out=yb[:, 0:512], in_=pb[:, :]), reads=[pbk], writes=[ybk])
                else:
                    CP("dve", yb[:, 512:1024], pb[:, :], [pbk], [ybk])
            DMA("sp", ys_d[row0:row0 + 128, :], yb[:], [ybk], ["ys_d"], "yst%d" % (bi % 2))
            if b == CAP // 128 - 1 and e_ + NSLOT < NE_run:
                issue_w(e_ + NSLOT)

        for e_ in range(min(NSLOT, NE_run)):
            issue_w(e_)
        if NB > 0:
            stA(0)
        if NB > 1:
            stA(1)
        if NB > 0:
            stB(0)
        for bi in range(NB):
            if bi + 2 < NB:
                stA(bi + 2)
            if bi >= 1:
                stC2(bi - 1)
            if bi + 1 < NB:
                stB(bi + 1)
            stC1(bi)
        if NB > 0:
            stC2(NB - 1)

        for ti in range(NT if stage >= 6 else 0):
            xrb = xr[ti % 2]
            xrk = "xr%d" % (ti % 2)
            if ti == 0:
                DMA("sp", xrb[:], x2_d[0:128, :], ["x2d0"], [xrk], xrk)
            if ti + 1 < NT:
                DMA("sp", xr[(ti + 1) % 2][:], x2_d[(ti + 1) * 128:(ti + 2) * 128, :], ["x2d%d" % (ti + 1)], ["xr%d" % ((ti + 1) % 2)], "xr%d" % ((ti + 1) % 2))
            y1, y1k = (xt[0], "xt0") if ti % 2 == 0 else (bc[:, G_CROSS:G_CROSS + 1024], "g_cross")
            y2, y2k = (xt[1], "xt1") if ti % 2 == 0 else (bc[:, G_FFN:G_FFN + 1024], "g_ffn")
            igA = ig0 if ti % 2 == 0 else ig2
            igB = ig1 if ti % 2 == 0 else ig3
            igAk = "ig0" if ti % 2 == 0 else "ig2"
            igBk = "ig1" if ti % 2 == 0 else "ig3"
            CP("dve", igA[:], di[:, ti, 0:1], ["di"], [igAk])
            CP("dve", igB[:], di[:, ti, 1:2], ["di"], [igBk])
            S.add("pool", lambda e, y1=y1, igA=igA: e.indirect_dma_start(out=y1[:, :], out_offset=None, in_=ys_d,
                                                         in_offset=bass.IndirectOffsetOnAxis(ap=igA[:, :], axis=0), bounds_check=breg(e), oob_is_err=False),
                  reads=[igAk, "ys_d"], writes=[y1k], dma=y1k)
            S.add("pool", lambda e, y2=y2, igB=igB: e.indirect_dma_start(out=y2[:, :], out_offset=None, in_=ys_d,
                                                         in_offset=bass.IndirectOffsetOnAxis(ap=igB[:, :], axis=0), bounds_check=breg(e), oob_is_err=False),
                  reads=[igBk, "ys_d"], writes=[y2k], dma=y2k)
            STT(xrb[:], y1[:], gt[:, ti, 0:1], xrb[:], ALU.mult, ALU.add, [y1k, "gt", xrk], [xrk])
            STT(xrb[:], y2[:], gt[:, ti, 1:2], xrb[:], ALU.mult, ALU.add, [y2k, "gt", xrk], [xrk])
            ACT(junk[:], xrb[:], AF.Square, [xrk], ["ss"], accum=st8[:, 0:1])
            rstd_from_ss(0, 1024, ["ss"], ["ss"])
            STT(xrb[:], xrb[:], st8[:, 0:1], bc[:, G_MIX:G_MIX + 1024], ALU.mult, ALU.mult, [xrk, "ss", "g_mix"], [xrk])
            DMA("sp", outd[ti * 128:(ti + 1) * 128, :], xrb[:], [xrk], ["out%d" % ti], "ost%d" % (ti % 2))
        fin = ["out%d" % ti for ti in range(NT if stage >= 6 else 0)] + ["KcT", "Vc", "CN0", "CNb3"]
        if dbg:
            fin += list(dbgd.keys())
        S.add("sp", None, reads=fin)

        S.finalize()
        sems = {e: [es.enter_context(nc.semaphore("sem_%s_%d" % (e, i))) for i in range(S.nsig[e] // EPOCH + 1)] for e in ENGS}
        dma_sems = {k: es.enter_context(nc.semaphore("ds%d" % i)) for i, k in enumerate(S.dma_cnt)}
        with nc.Block() as block:
            @block.sync
            def _(e):
                S.emit_engine("sp", e, sems, dma_sems)
                for k_, c_ in S.dma_cnt.items():
                    e.wait_ge(dma_sems[k_], c_)

            @block.scalar
            def _(e):
                S.emit_engine("act", e, sems, dma_sems)

            @block.vector
            def _(e):
                S.emit_engine("dve", e, sems, dma_sems)

            @block.gpsimd
            def _(e):
                S.emit_engine("pool", e, sems, dma_sems)

            @block.tensor
            def _(e):
                S.emit_engine("pe", e, sems, dma_sems)
    return nc, S


def host_inputs(inp):
    f = np.float32
    x = np.asarray(inp["x"], f)
    mem = np.asarray(inp["mem"], f)
    bcrow = np.concatenate([
        np.asarray(inp["norm_mix"], f)[0], np.asarray(inp["norm_cross"], f)[0], np.asarray(inp["norm_ffn"], f)[0],
        np.concatenate([np.asarray(inp["norm_att_out"], f)[0], np.asarray(inp["norm_ml_out"], f)[0]]),
        np.asarray(inp["norm_final"], f), np.asarray(inp["norm_mem"], f)[0],
        np.asarray(inp["b_router_group"], f)[0], np.asarray(inp["b_router_expert"], f)[0], np.asarray(inp["att_sinks"], f)[0],
        np.zeros(128 - 44, f)])
    bcd = np.ascontiguousarray(np.broadcast_to(bcrow[None, :], (128, bcrow.shape[0])))
    w_r = np.ascontiguousarray(np.concatenate([np.asarray(inp["w_router_group"], f)[0], np.asarray(inp["w_router_expert"], f)[0]], axis=1))
    s_ = np.arange(128)[:, None]
    t_ = np.arange(128)[None, :]
    m_prev = (s_ > t_).astype(f)
    m_cur = (s_ <= t_).astype(f)
    tri_lt = (s_ < t_).astype(f)
    slopes = np.exp2(-8.0 * np.arange(1, 9, dtype=np.float64) / 8).astype(f)
    common = dict(
        w_in=np.ascontiguousarray(np.asarray(inp["w_in"], f)[0]), w_out=np.ascontiguousarray(np.asarray(inp["w_out"], f)[0]),
        w_cq=np.ascontiguousarray(np.asarray(inp["w_cq"], f)[0]), w_ckv=np.ascontiguousarray(np.asarray(inp["w_ckv"], f)[0]),
        w_co=np.ascontiguousarray(np.asarray(inp["w_co"], f)[0]), w_r=w_r,
        w_eg=np.ascontiguousarray(np.asarray(inp["w_e_gate"], f)[0]), w_eu=np.ascontiguousarray(np.asarray(inp["w_e_up"], f)[0]),
        w_ed=np.ascontiguousarray(np.asarray(inp["w_e_down"], f)[0]), bcd=bcd)
    conv_w = np.asarray(inp["conv_w"], f)[0]
    conv_b = np.asarray(inp["conv_b"], f)[0]
    b_g = np.asarray(inp["b_gates"], f)[0]
    maps = []
    for c in range(8):
        b, half = c // 2, c % 2
        pp = np.zeros((128, 128), f)
        pp[:, 0:32] = conv_w.reshape(4, 8, 128).transpose(2, 1, 0).reshape(128, 32)
        pp[:, 32:40] = conv_b.reshape(8, 128).T
        pp[0:4, 40] = b_g[0:4]
        pp[0:4, 41] = b_g[4:8]
        pp[:, 42] = 1.0 if half == 1 else 0.0
        pp[:, 43] = 0.0 if half == 1 else -30000.0
        for kb in range(2):
            for h in range(8):
                pp[:, 44 + kb * 8 + h] = slopes[h] * (np.arange(128) + kb * 128 - 255.0)
        for h in range(8):
            pp[:, 60 + h] = slopes[h] * (np.arange(128) + 128 - 255.0)
        pp[:, 68:100] = (np.arange(32) * CAP)[None, :]
        cm = np.zeros((128, 5, 128), f)
        cm[:, 0] = np.eye(128, dtype=f)
        cm[:, 1] = m_prev
        cm[:, 2] = m_cur
        cm[:, 3] = m_prev if half == 1 else 0.0
        cm[:, 4] = tri_lt
        d = dict(common)
        d["xo"] = np.ascontiguousarray(x[b, half * 2048:(half + 1) * 2048])
        d["xp"] = np.ascontiguousarray(x[b, 0:2048]) if half == 1 else np.zeros((2048, 1024), f)
        d["memb"] = np.ascontiguousarray(mem[b])
        d["ppd"] = pp
        d["cmd"] = cm
        maps.append(d)
    return maps


_NC_CACHE = {}


def kernel(**inputs):
    if "nc" not in _NC_CACHE:
        _NC_CACHE["nc"] = build(False)[0]
    nc = _NC_CACHE["nc"]
    maps = host_inputs(inputs)
    res = run_bass_kernel_spmd(nc, maps, core_ids=list(range(8)))
    out = np.zeros((4, 4096, 1024), np.float32)
    for c in range(8):
        b, half = c // 2, c % 2
        out[b, half * 2048:(half + 1) * 2048] = res.results[c]["out"]
    return out
```

```python
import math
import os
from contextlib import ExitStack

import numpy as np
import concourse.bass as bass
import concourse.mybir as mybir
from concourse.bass_utils import run_bass_kernel_spmd

F32 = mybir.dt.float32
BF16 = mybir.dt.bfloat16
I32 = mybir.dt.int32
AF = mybir.ActivationFunctionType
ALU = mybir.AluOpType
AX = mybir.AxisListType

ENGS = ["pe", "act", "dve", "pool", "sp"]
NT = 16
NG = 8
GT = 256
NE = 32
CAP = 256
EPS = 1e-6
LN_SQRT_DH = 0.5 * math.log(128.0)
EPOCH = 400


class Op:
    __slots__ = ("eng", "emit", "deps", "is_dma", "semkey", "count", "signal", "nosync", "depcnt", "idx")


class Sched:
    def __init__(self):
        self.ops = {e: [] for e in ENGS}
        self.last_w = {}
        self.readers = {}
        self.dma_cnt = {}
        self.dma_last = {}
        self.nsig = {}

    def add(self, eng, emit, reads=(), writes=(), dma=None, nosync=False):
        op = Op()
        op.eng = eng
        op.emit = emit
        op.is_dma = dma is not None
        op.semkey = dma
        op.signal = False
        op.count = 0
        op.nosync = nosync
        op.idx = len(self.ops[eng])
        deps = []
        for r in reads:
            w = self.last_w.get(r)
            if w is not None:
                deps.append(w)
        for k in writes:
            w = self.last_w.get(k)
            if w is not None:
                deps.append(w)
            deps.extend(self.readers.get(k, ()))
        own_before = 0
        if op.is_dma:
            p = self.dma_last.get(dma)
            if p is not None and not nosync:
                deps.append(p)
            self.dma_last[dma] = op
            own_before = self.dma_cnt.get(dma, 0)
            self.dma_cnt[dma] = own_before + 16
            op.count = self.dma_cnt[dma]
        seen = set()
        od = []
        for d in deps:
            if id(d) in seen or d is op:
                continue
            seen.add(id(d))
            if (not d.is_dma) and (not op.is_dma) and d.eng == "pe" and eng == "pe":
                continue
            od.append(d)
        latest = {}
        for d in od:
            if not d.is_dma:
                if d.eng not in latest or d.idx > latest[d.eng].idx:
                    latest[d.eng] = d
        od = [d for d in od if d.is_dma or latest[d.eng] is d]
        for d in od:
            if not d.is_dma:
                d.signal = True
        op.deps = od
        op.depcnt = [((own_before if (op.is_dma and d.semkey == op.semkey) else self.dma_cnt[d.semkey]) if (d.is_dma and d.nosync) else None) for d in od]
        for r in reads:
            self.readers.setdefault(r, []).append(op)
        for k in writes:
            self.last_w[k] = op
            self.readers[k] = []
        self.ops[eng].append(op)
        return op

    def finalize(self):
        for e in ENGS:
            c = 0
            for op in self.ops[e]:
                if op.is_dma:
                    continue
                if op.signal:
                    op.count = c
                    c += 1
            self.nsig[e] = c

    def emit_engine(self, eng, handle, sems, dma_sems):
        seen = {}
        for op in self.ops[eng]:
            need = {}
            for d, dc in zip(op.deps, op.depcnt):
                if d.is_dma:
                    key = ("d", d.semkey)
                    cnt_ = d.count if dc is None else dc
                else:
                    key = ("e", d.eng)
                    cnt_ = d.count + 1
                if cnt_ > need.get(key, 0):
                    need[key] = cnt_
            for key, cnt in need.items():
                if seen.get(key, 0) >= cnt:
                    continue
                seen[key] = cnt
                if key[0] == "d":
                    handle.wait_ge(dma_sems[key[1]], cnt)
                else:
                    ep, v = (cnt - 1) // EPOCH, (cnt - 1) % EPOCH + 1
                    handle.wait_ge(sems[key[1]][ep], v)
            if op.emit is None:
                continue
            ins = op.emit(handle)
            if op.is_dma:
                ins.then_inc(dma_sems[op.semkey], 16)
            elif op.signal:
                ins.then_inc(sems[eng][op.count // EPOCH], 1)


def build(dbg=False, stage=9):
    nc = bass.Bass("TRN2", target_bir_lowering=False)
    S = Sched()

    def din(name, shape, dt=F32):
        return nc.dram_tensor(name, shape, dt, kind="ExternalInput").ap()

    xo = din("xo", [2048, 1024])
    xp = din("xp", [2048, 1024])
    memb = din("memb", [256, 1024])
    w_in = din("w_in", [1024, 2824])
    w_out = din("w_out", [1024, 1024])
    w_cq = din("w_cq", [1024, 512])
    w_ckv = din("w_ckv", [1024, 1024])
    w_co = din("w_co", [512, 1024])
    w_r = din("w_r", [1024, 36])
    NEd = NE if stage >= 5 else 1
    w_eg = din("w_eg", [NEd, 1024, 512])
    w_eu = din("w_eu", [NEd, 1024, 512])
    w_ed = din("w_ed", [NEd, 512, 1024])
    bcd = din("bcd", [128, 6 * 1024 + 128])
    ppd = din("ppd", [128, 128])
    cmd = din("cmd", [128, 5, 128])
    outd = nc.dram_tensor("out", [2048, 1024], F32, kind="ExternalOutput").ap()
    xs_d = nc.dram_tensor("xs_d", [NE * CAP + 128, 1024], BF16, kind="Internal").ap()
    ys_d = nc.dram_tensor("ys_d", [NE * CAP + 128, 1024], F32, kind="Internal").ap()
    x2_d = nc.dram_tensor("x2_d", [2048, 1024], F32, kind="Internal").ap()
    dbgd = {}
    if dbg:
        for nm, sh in [("d_att", [2048, 512]), ("d_hm", [2048, 512]), ("d_x1", [2048, 1024]),
                       ("d_x2", [2048, 1024]), ("d_lg", [2048, 36]), ("d_di", [2048, 2]),
                       ("d_gt", [2048, 2])]:
            dbgd[nm] = nc.dram_tensor(nm, sh, F32, kind="ExternalOutput").ap()

    es = ExitStack()
    with es:
        def sb(name, shape, dt=F32):
            return es.enter_context(nc.sbuf_tensor(name, shape, dt))

        def psum(name, shape, dt=F32):
            return es.enter_context(nc.psum_tensor(name, shape, dt))

        BR = {}

        def breg(e):
            if 'r' not in BR:
                BR['r'] = e.to_reg(NE * CAP + 127)
            return BR['r']

        def DMA(eng, out, in_, r, w, key, nosync=False):
            S.add(eng, lambda e: e.dma_start(out=out, in_=in_), reads=r, writes=w, dma=key, nosync=nosync)

        def MM(out, lhsT, rhs, st, sp_, r, w):
            S.add("pe", lambda e: e.matmul(out, lhsT=lhsT, rhs=rhs, start=st, stop=sp_), reads=r, writes=w)

        def TR(out, in_, ident, r, w):
            S.add("pe", lambda e: e.transpose(out=out, in_=in_, identity=ident), reads=r, writes=w)

        def ACT(out, in_, func, r, w, bias=None, scale=None, accum=None):
            kw = {}
            if bias is not None:
                kw["bias"] = bias
            if scale is not None:
                kw["scale"] = scale
            if accum is not None:
                kw["accum_out"] = accum
            S.add("act", lambda e: e.activation(out=out, in_=in_, func=func, **kw), reads=r, writes=w)

        def TS(eng, out, in0, s1, s2, op0, op1, r, w):
            if op1 is None:
                S.add(eng, lambda e: e.tensor_scalar(out=out, in0=in0, scalar1=s1, scalar2=None, op0=op0), reads=r, writes=w)
            else:
                S.add(eng, lambda e: e.tensor_scalar(out=out, in0=in0, scalar1=s1, scalar2=s2, op0=op0, op1=op1), reads=r, writes=w)

        def STT(out, in0, scalar, in1, op0, op1, r, w, accum=None):
            if accum is None:
                S.add("dve", lambda e: e.scalar_tensor_tensor(out=out, in0=in0, scalar=scalar, in1=in1, op0=op0, op1=op1), reads=r, writes=w)
            else:
                S.add("dve", lambda e: e.scalar_tensor_tensor(out=out, in0=in0, scalar=scalar, in1=in1, op0=op0, op1=op1, accum_out=accum), reads=r, writes=w)

        def TT(eng, out, in0, in1, op, r, w):
            S.add(eng, lambda e: e.tensor_tensor(out=out, in0=in0, in1=in1, op=op), reads=r, writes=w)

        def CP(eng, out, in_, r, w):
            S.add(eng, lambda e: e.tensor_copy(out=out, in_=in_), reads=r, writes=w)

        def MSET(eng, ap, val, w):
            S.add(eng, lambda e: e.memset(ap, val), writes=w)

        def RECIP(out, in_, r, w):
            S.add("dve", lambda e: e.reciprocal(out=out, in_=in_), reads=r, writes=w)

        def SCAN(out, d0, d1, init, op0, op1, r, w):
            S.add("dve", lambda e: e.tensor_tensor_scan(out=out, data0=d0, data1=d1, initial=init, op0=op0, op1=op1), reads=r, writes=w)

        ARN = 41024
        arena = sb("arena", [128, ARN], BF16)
        win = arena[:, 0:22592].rearrange("p (r c) -> p r c", r=8)
        wout = arena[:, 22592:30784].rearrange("p (r c) -> p r c", r=8)
        wckv = wout
        wcq = arena[:, 30784:34880].rearrange("p (r c) -> p r c", r=8)
        wco = arena[:, 34880:38976].rearrange("p (r c) -> p r c", r=4)
        wkd = arena[:, 38976:41024].rearrange("p (r c) -> p r c", r=8)
        bc = sb("bc", [128, 4 * 1024 + 128])
        G_MIX, G_CROSS, G_FFN, G_AM, MISC = 0, 1024, 2048, 3072, 4096
        pp = sb("pp", [128, 128])
        cmf = sb("cmf", [128, 5, 128])
        identf = cmf[:, 0, :]
        cmb = sb("cmb", [128, 5, 128], BF16)
        identb = cmb[:, 0, :]
        wr = sb("wr", [128, 8, 36], BF16)
        KcT = sb("KcT", [128, 4, 256], BF16)
        Vc = sb("Vc", [128, 2, 4, 129], BF16)
        CN = sb("CN", [128, 4, 129])
        CNb = sb("CNb", [128, 4, 129], BF16)
        stg = sb("stg", [128, 8, 3 + GT])
        gt = sb("gt", [128, NT, 2])
        di = sb("di", [128, NT, 2], I32)
        Acum = sb("Acum", [128, NE], BF16)
        sinkt = sb("sinkt", [128, 8])
        ones_b = sb("ones_b", [128, 128], BF16)
        ones4 = sb("ones4", [4, 128])
        qaT = [sb("qaT%d" % i, [128, 4, GT], BF16) for i in range(2)]
        kaT = [sb("kaT%d" % i, [128, 2, 128 + GT], BF16) for i in range(2)]
        va = [sb("va%d" % i, [128, 3, 2, 65], BF16) for i in range(2)]
        qkT = [sb("qkT%d" % i, [128, 8, GT], BF16) for i in range(2)]
        vm = [sb("vm%d" % i, [128, 2, 4, 129], BF16) for i in range(2)]
        so = [sb("so%d" % i, [128, 2, 512], BF16) for i in range(2)]
        gsc = [sb("gsc%d" % i, [128, 2, 2, 4]) for i in range(2)]
        dcy = [sb("dcy%d" % i, [128, 2, 4]) for i in range(2)]
        xt = [sb("xt%d" % i, [128, 1024]) for i in range(2)]
        xr = [sb("xr%d" % i, [128, 1024]) for i in range(2)]
        bfA = [sb("bfA%d" % i, [128, 1024], BF16) for i in range(3)]
        tpA = [sb("tpA%d" % i, [128, 8, 128], BF16) for i in range(2)]
        hT = sb("hT", [128, 8, GT], BF16)
        junk = sb("junk", [128, 1024], BF16)
        cacc = [sb("cacc%d" % i, [128, GT]) for i in range(2)]
        st8 = sb("st8", [128, 32])
        e1 = sb("e1", [4, GT])
        sp1 = sb("sp1", [4, GT])
        Bn = sb("Bn", [4, 1 + GT])
        ug = sb("ug", [4, GT])
        Ub = sb("Ub", [4, 1 + GT])
        BnC = sb("BnC", [4, 1])
        UC = sb("UC", [4, 1])
        nb = sb("nb", [4, 4])
        scT = sb("scT", [4, 2, GT])
        dx = sb("dx", [4, 2, 4])
        zer4 = sb("zer4", [4, GT])
        one4 = sb("one4", [4, GT])
        nbf = sb("nbf", [4, 1])
        PT = [sb("PT%d" % i, [128, 512], BF16) for i in range(2)]
        smT = [sb("smT%d" % i, [128, 128], BF16) for i in range(2)]
        kp = [sb("kp%d" % i, [128, 128], BF16) for i in range(2)]
        att_o = sb("att_o", [128, 512])
        hmix = sb("hmix", [128, 512])
        qcT = sb("qcT", [128, 4, 128], BF16)
        ocp = sb("ocp", [128, 512], BF16)
        ocT = sb("ocT", [128, 4, 128], BF16)
        lg = sb("lg", [128, 36])
        rt = sb("rt", [128, 96])
        A1 = sb("A1", [128, NE])
        A2 = sb("A2", [128, NE])
        Ab = sb("Ab", [128, NE], BF16)
        cbuf = sb("cbuf", [128, NE])
        ig0 = sb("ig0", [128, 1], I32)
        ocpB = sb("ocpB", [128, 512], BF16)
        ocTB = sb("ocTB", [128, 4, 128], BF16)
        mixb = [sb("mixb%d" % i, [128, 1024], BF16) for i in range(2)]
        ybuf = sb("ybuf", [128, 1024], BF16)
        PcT = [sb("PcT%d" % i, [128, 512], BF16) for i in range(2)]
        ig1 = sb("ig1", [128, 1], I32)
        ig2 = sb("ig2", [128, 1], I32)
        ig3 = sb("ig3", [128, 1], I32)

        T0 = psum("T0", [128, 8, 128], BF16)
        T1 = psum("T1", [128, 8, 128], BF16)
        PA = psum("PA", [128, 512])
        PB = psum("PB", [128, 512])
        PS_ = psum("PS_", [128, 512])
        PV = psum("PV", [128, 512])
        PF = psum("PF", [128, 512])
        PG = psum("PG", [128, 512])
        projbanks = [(PA, "PA"), (PB, "PB")]
        pbi = [0]

        def nextbank():
            b = projbanks[pbi[0] % 2]
            pbi[0] += 1
            return b

        DMA("sp", pp[:], ppd, [], ["pp"], "pp")
        DMA("sp", cmf[:], cmd, [], ["cmf"], "cmf")
        DMA("sp", bc[:, 0:4096], bcd[:, 0:4096], [], ["g_mix", "g_cross", "g_ffn", "g_am"], "bc0")
        DMA("sp", bc[:, 4096:4224], bcd[:, 6144:6272], [], ["misc"], "bc1")
        DMA("sp", xr[1][:], bcd[:, 5120:6144], [], ["xr1"], "xr1")
        CP("dve", cmb[:], cmf[:], ["cmf"], ["cmb"])
        MSET("pool", ones_b[:], 1.0, ["ones_b"])
        MSET("pool", hT[:, 0:4, :], 0.0, ["hT"])
        NZB = (NE * CAP + 128) // 128
        xs_v = xs_d.rearrange("(n p) c -> p n c", p=128)
        for z0 in range(0, NZB, 13):
            z1 = min(NZB, z0 + 13)
            DMA("sp", xs_v[:, z0:z1, :], hT[:, 0:4, :].rearrange("p r c -> p (r c)").unsqueeze(1).to_broadcast([128, z1 - z0, 1024]), ["hT"], ["xs_d"], "xsz", nosync=True)
        print("sbuf remaining", nc.sbuf_bytes_remaining)
        MSET("pool", ones4[:], 1.0, ["ones4"])
        MSET("pool", zer4[:], 0.0, ["zer4"])
        MSET("pool", one4[:], 1.0, ["one4"])
        MSET("pool", stg[:], 0.0, ["stg%d" % c for c in range(8)])
        MSET("pool", BnC[:], 0.0, ["BnC"])
        MSET("pool", UC[:], 0.0, ["UC"])
        MSET("pool", Acum[:], 0.0, ["Acum"])
        MSET("pool", CN[:], 0.0, ["CN%d" % h for h in range(4)])
        for i in range(2):
            MSET("pool", va[i][:, :, :, 64:65], 1.0, ["va%d" % i])
            MSET("pool", vm[i][:, :, :, 128:129], 1.0, ["vm%d" % i])
        MSET("pool", Vc[:, :, :, 128:129], 1.0, ["Vc"])
        PP_CW, PP_CB, PP_BI, PP_BF, PP_FLAG, PP_PM, PP_AB, PP_CQ, PP_BASE = 0, 32, 40, 41, 42, 43, 44, 60, 68
        cw = pp[:, PP_CW:PP_CW + 32].rearrange("p (c j) -> p c j", j=4)
        TS("dve", nbf[:], pp[0:4, PP_BF:PP_BF + 1], -1.0, None, ALU.mult, None, ["pp"], ["nbf"])
        TT("dve", sinkt[:], pp[:, PP_CQ:PP_CQ + 8], bc[:, MISC + 36:MISC + 44], ALU.add, ["pp", "misc"], ["sinkt"])
        ACT(sinkt[:], sinkt[:], AF.Exp, ["sinkt"], ["sinkt"])

        def wload(dst2d, src2d, ncols, key, rkeys):
            c0 = 0
            i = 0
            while c0 < ncols:
                c1 = min(ncols, c0 + 2048)
                DMA("pool", dst2d[:, c0:c1], src2d[:, c0:c1], [], [rkeys[i]], key, nosync=True)
                c0 = c1
                i += 1

        WIN_K = ["win%d" % i for i in range(12)]
        WOUT_K = ["wout%d" % i for i in range(4)]
        WCQ_K = ["wcq%d" % i for i in range(2)]
        WCO_K = ["wco%d" % i for i in range(2)]
        wload(arena[:, 22592:30784], w_ckv.rearrange("(p r) c -> p (r c)", r=8), 8192, "wckv", WOUT_K)
        wload(arena[:, 0:22592], w_in.rearrange("(p r) c -> p (r c)", r=8), 22592, "win", WIN_K)
        S.add("pool", lambda e: e.dma_start(out=wr[:].rearrange("p r c -> p (r c)"), in_=w_r.rearrange("(p r) c -> p (r c)", r=8)), writes=["wr"], dma="wr")

        def rstd_from_ss(col, nfeat, r, w):
            ACT(st8[:, col:col + 1], st8[:, col:col + 1], AF.Ln, r, w, bias=EPS, scale=1.0 / nfeat)
            ACT(st8[:, col:col + 1], st8[:, col:col + 1], AF.Exp, w, w, scale=-0.5)

        def norm_to_bf(src, srckeys, gain, gkey, dst, dstkey, R=8):
            ACT(junk[:], src, AF.Square, srckeys, ["ss"], accum=st8[:, 0:1])
            rstd_from_ss(0, 1024, ["ss"], ["ss"])
            STT(dst, src, st8[:, 0:1], gain, ALU.mult, ALU.mult, srckeys + ["ss", gkey], [dstkey])

        def transpose8(src_bf, srckey, dstT, dstkey, nchunk=8, bank=None, bankkey=None):
            bank = T0 if bank is None else bank
            bankkey = "T0" if bankkey is None else bankkey
            for r in range(nchunk):
                TR(bank[:, r, :], src_bf[:, r:128 * nchunk:nchunk], identb, [srckey, "cmb"], [bankkey])
            S.add("act", lambda e: e.copy(out=dstT, in_=bank[:, 0:nchunk, :]), reads=[bankkey], writes=[dstkey])

        for mt in range(2):
            DMA("sp", xt[mt][:], memb[mt * 128:(mt + 1) * 128, :], [], ["xt%d" % mt], "xt%d" % mt)
            norm_to_bf(xt[mt][:], ["xt%d" % mt], xr[1][:], "xr1", bfA[mt][:], "bfA%d" % mt)
            transpose8(bfA[mt][:], "bfA%d" % mt, tpA[mt][:], "tpA%d" % mt)
        for h in range(4):
            bk, bkk = nextbank()
            for mt in range(2):
                for r in range(8):
                    MM(bk[:, mt * 128:(mt + 1) * 128], wckv[:, r, h * 128:(h + 1) * 128], tpA[mt][:, r, :], r == 0, r == 7,
                       WOUT_K + ["tpA%d" % mt], [bkk])
            CP("dve", KcT[:, h, :], bk[:, 0:256], [bkk], ["KcT"])
        for mt in range(2):
            bk, bkk = nextbank()
            for r in range(8):
                MM(bk[:, :], tpA[mt][:, r, :], wckv[:, r, 512:1024], r == 0, r == 7, WOUT_K + ["tpA%d" % mt], [bkk])
            CP("dve", Vc[:, mt, :, 0:128], bk[:, :].rearrange("p (h d) -> p h d", h=4), [bkk], ["Vc"])
        wload(arena[:, 22592:30784], w_out.rearrange("(p r) c -> p (r c)", r=8), 8192, "wout", WOUT_K)
        wload(arena[:, 30784:34880], w_cq.rearrange("(p r) c -> p (r c)", r=8), 4096, "wcq", WCQ_K)
        wload(arena[:, 34880:38976], w_co.rearrange("(p r) c -> p (r c)", r=4), 4096, "wco", WCO_K)
        for j in range(4):
            kv = j // 2
            CP("pool", wkd[:, :, j * 64:(j + 1) * 64], win[:, :, 512 + kv * 64:512 + (kv + 1) * 64], WIN_K, ["wkd"])

        def load_norm_transpose2(src, t0):
            T1K = ["T1_0", "T1_1", "T1_2", "T1_3", "T1o"]
            xb = [xt[(t0 + j) % 2] for j in range(2)]
            xk = ["xt%d" % ((t0 + j) % 2) for j in range(2)]
            bf = [bfA[(t0 + j) % 3] for j in range(2)]
            bfk = ["bfA%d" % ((t0 + j) % 3) for j in range(2)]
            col = [0, 16]
            ssk = ["ss", "ss2"]
            for j in range(2):
                DMA("sp", xb[j][:], src[(t0 + j) * 128:(t0 + j + 1) * 128, :], [], [xk[j]], xk[j])
            for j in range(2):
                ACT(junk[:], xb[j][:], AF.Square, [xk[j]], [ssk[j]], accum=st8[:, col[j]:col[j] + 1])
                ACT(st8[:, col[j]:col[j] + 1], st8[:, col[j]:col[j] + 1], AF.Ln, [ssk[j]], [ssk[j]], bias=EPS, scale=1.0 / 1024)
                ACT(st8[:, col[j]:col[j] + 1], st8[:, col[j]:col[j] + 1], AF.Exp, [ssk[j]], [ssk[j]], scale=-0.5)
            for j in range(2):
                STT(bf[j][:], xb[j][:], st8[:, col[j]:col[j] + 1], bc[:, G_MIX:G_MIX + 1024], ALU.mult, ALU.mult, [xk[j], ssk[j], "g_mix"], [bfk[j]])
            for r in range(8):
                TR(T0[:, r, :], bf[0][:, r:1024:8], identb, [bfk[0], "cmb"], ["T0"])
            for r in range(8):
                TR(T1[:, r, :], bf[1][:, r:1024:8], identb, [bfk[1], "cmb"], T1K)
            S.add("act", lambda e: e.copy(out=hT[:, :, 0:128], in_=T0[:, :, :]), reads=["T0"], writes=["hT"])
            S.add("act", lambda e: e.copy(out=hT[:, :, 128:256], in_=T1[:, :, :]), reads=T1K, writes=["hT"])

        def fm_proj(wsel, wkeys, evac):
            bk, bkk = nextbank()
            for r in range(8):
                MM(bk[:, 0:GT], wsel(r), hT[:, r, :], r == 0, r == 7, wkeys + ["hT"], [bkk])
            evac(bk, bkk)

        def conv_chunk(c, gp):
            def ev(bk, bkk):
                sk = "stg%d" % c
                CP("pool", stg[:, c, 0:3], stg[:, c, GT:GT + 3], [sk], [sk])
                S.add("act", lambda e: e.copy(out=stg[:, c, 3:3 + GT], in_=bk[:, 0:GT]), reads=[bkk], writes=[sk])
                flush_silu()
                ca = cacc[c % 2]
                cak = "cacc%d" % (c % 2)
                TS("dve", ca[:], stg[:, c, 0:GT], cw[:, c, 0:1], None, ALU.mult, None, [sk, "pp"], [cak])
                for jj in range(1, 4):
                    STT(ca[:], stg[:, c, jj:jj + GT], cw[:, c, jj:jj + 1], ca[:], ALU.mult, ALU.add, [sk, "pp", cak], [cak])
                PEND.append(lambda: ACT(qkT[gp][:, c, :], ca[:], AF.Silu, [cak, "pp"], ["qkT%d_%d" % (gp, c)], bias=pp[:, PP_CB + c:PP_CB + c + 1]))
            fm_proj(lambda r: win[:, r, 768 + c * 128:768 + (c + 1) * 128], WIN_K, ev)

        PEND = []

        def flush_silu():
            while PEND:
                PEND.pop(0)()

        def gates_group(gp, prefix, first):
            bk, bkk = nextbank()
            for r in range(8):
                MM(bk[0:4, 0:GT], win[:, r, 2816:2820], hT[:, r, :], r == 0, r == 7, WIN_K + ["hT"], [bkk])
            TS("dve", ug[:], bk[0:4, 0:GT], pp[0:4, PP_BI:PP_BI + 1], None, ALU.add, None, [bkk, "pp"], ["ug"])
            bk2, bkk2 = nextbank()
            for r in range(8):
                MM(bk2[0:4, 0:GT], win[:, r, 2820:2824], hT[:, r, :], r == 0, r == 7, WIN_K + ["hT"], [bkk2])
            ACT(e1[:], bk2[0:4, 0:GT], AF.Exp, [bkk2, "nbf"], ["e1"], bias=nbf[:], scale=-1.0)
            yield
            ACT(sp1[:], e1[:], AF.Ln, ["e1"], ["sp1"], bias=1.0)
            yield
            if prefix:
                TS("dve", sp1[:], sp1[:], pp[0:4, PP_FLAG:PP_FLAG + 1], None, ALU.mult, None, ["sp1", "pp"], ["sp1"])
            SCAN(Bn[:, 1:1 + GT], one4[:], sp1[:], BnC[:], ALU.mult, ALU.add, ["one4", "sp1", "BnC", "Bn"], ["Bn"])
            yield
            CP("dve", BnC[:], Bn[:, GT:GT + 1], ["Bn"], ["BnC"])
            TT("dve", ug[:], ug[:], Bn[:, 1:1 + GT], ALU.add, ["ug", "Bn"], ["ug"])
            yield
            if prefix:
                TS("dve", ug[:], ug[:], pp[0:4, PP_FLAG:PP_FLAG + 1], pp[0:4, PP_PM:PP_PM + 1], ALU.mult, ALU.add, ["ug", "pp"], ["ug"])
            CP("dve", Ub[:, 0:1], UC[:], ["UC"], ["Ub"])
            yield
            SCAN(Ub[:, 1:1 + GT], ug[:], zer4[:], UC[:], ALU.max, ALU.max, ["ug", "zer4", "UC", "Ub"], ["Ub"])
            yield
            CP("dve", UC[:], Ub[:, GT:GT + 1], ["Ub"], ["UC"])
            TS("dve", nb[:, 0:2], Ub[:, 0:GT:128], -1.0, -LN_SQRT_DH, ALU.mult, ALU.add, ["Ub"], ["nb"])
            TS("dve", nb[:, 2:4], Ub[:, 0:GT:128], -1.0, None, ALU.mult, None, ["Ub"], ["nb"])
            yield
            for cc in range(2):
                ACT(scT[:, 0, cc * 128:(cc + 1) * 128], ug[:, cc * 128:(cc + 1) * 128], AF.Exp, ["ug", "nb"], ["scT"], bias=nb[:, cc:cc + 1])
                if not prefix:
                    ACT(scT[:, 1, cc * 128:(cc + 1) * 128], Bn[:, 1 + cc * 128:1 + (cc + 1) * 128], AF.Exp, ["Bn", "nb"], ["scT"], bias=nb[:, 2 + cc:3 + cc])
                    yield
            TT("dve", nb[:, 0:2], Ub[:, 0:GT:128], Ub[:, 128:GT + 1:128], ALU.subtract, ["Ub", "nb", "scT"], ["nb"])
            yield
            ACT(nb[:, 0:2], nb[:, 0:2], AF.Exp, ["nb"], ["nb"])
            yield
            TT("dve", dx[:], nb[:, 0:2].unsqueeze(2).to_broadcast([4, 2, 4]), identf[0:4, 0:4].unsqueeze(1).to_broadcast([4, 2, 4]), ALU.mult, ["nb", "cmf"], ["dx"])
            yield
            bk3, bkk3 = nextbank()
            MM(bk3[:, 0:8], ones4[:, :], dx[:].rearrange("p c h -> p (c h)"), True, True, ["ones4", "dx"], [bkk3])
            CP("dve", dcy[gp][:].rearrange("p c h -> p (c h)"), bk3[:, 0:8], [bkk3], ["dcy%d" % gp])
            bk4, bkk4 = nextbank()
            nwh = 1 if prefix else 2
            for cc in range(2):
                for wh in range(nwh):
                    TR(bk4[:, (cc * 2 + wh) * 4:(cc * 2 + wh) * 4 + 4], scT[:, wh, cc * 128:(cc + 1) * 128], identf[0:4, 0:4], ["scT", "cmf"], [bkk4])
            if prefix:
                for cc in range(2):
                    CP("dve", gsc[gp][:, cc, 0, :], bk4[:, cc * 8:cc * 8 + 4], [bkk4], ["gsc%d" % gp])
            else:
                CP("dve", gsc[gp][:].rearrange("p c w h -> p (c w h)"), bk4[:, 0:16], [bkk4], ["gsc%d" % gp])

        def adv(gen, k):
            for _ in range(k):
                next(gen, None)

        def drain(gen):
            for _ in gen:
                pass

        def state_update(gp, j, h, first):
            kT = qkT[gp][:, 4 + h, j * 128:(j + 1) * 128]
            TR(T1[:, h, :], kT, identb, ["qkT%d_%d" % (gp, 4 + h), "cmb"], ["T1_%d" % h])
            kpb = kp[h % 2]
            kpk = "kp%d" % (h % 2)
            TS("dve", kpb[:], T1[:, h, :], gsc[gp][:, j, 0, h:h + 1], None, ALU.mult, None, ["T1_%d" % h, "gsc%d" % gp], [kpk])
            pf = PF if h % 2 == 0 else PG
            pfk = "PF" if h % 2 == 0 else "PG"
            MM(pf[:, 257:386], kpb[:], vm[gp][:, j, h, :], True, True, [kpk, "vm%d" % gp], [pfk])
            ck = "CN%d" % h
            if first:
                CP("dve", CN[:, h, :], pf[:, 257:386], [pfk], [ck])
            else:
                STT(CN[:, h, :], CN[:, h, :], st8[:, 4 + h:5 + h], pf[:, 257:386], ALU.mult, ALU.add, [pfk, ck, "dprev%d" % h], [ck])
            TS("dve", CNb[:, h, :], CN[:, h, :], dcy[gp][:, j, h:h + 1], None, ALU.mult, None, [ck, "dcy%d" % gp], ["CNb%d" % h])
            CP("dve", st8[:, 4 + h:5 + h], dcy[gp][:, j, h:h + 1], ["dcy%d" % gp, ck], ["dprev%d" % h])

        def prefix_proj(pg):
            gp = pg % 2
            load_norm_transpose2(xp, pg * 2)
            gg = gates_group(gp, True, pg == 0)
            adv(gg, 1)
            crange = range(8) if pg == NG - 1 else range(4, 8)
            for c in crange:
                conv_chunk(c, gp)
                adv(gg, 2)
            flush_silu()
            for j in range(2):
                bk, bkk = nextbank()
                for r in range(8):
                    MM(bk[:, :], hT[:, r, j * 128:(j + 1) * 128], win[:, r, 1792:2304], r == 0, r == 7, WIN_K + ["hT"], [bkk])
                CP("dve", vm[gp][:, j, :, 0:128], bk[:, :].rearrange("p (h d) -> p h d", h=4), [bkk], ["vm%d" % gp])
                adv(gg, 2)
            if pg == NG - 1:
                for kv in range(2):
                    def ev(bk, bkk, kv=kv):
                        S.add("act", lambda e: e.copy(out=kaT[gp][:, kv, 128:128 + GT], in_=bk[:, 0:GT]), reads=[bkk], writes=["kaT%d" % gp])
                    fm_proj(lambda r, kv=kv: wkd[:, r, kv * 128:(kv + 1) * 128], ["wkd"], ev)
                bk, bkk = nextbank()
                for r in range(8):
                    MM(bk[:, 0:128], hT[:, r, 128:256], win[:, r, 640:768], r == 0, r == 7, WIN_K + ["hT"], [bkk])
                CP("dve", va[gp][:, 2, :, 0:64], bk[:, 0:128].rearrange("p (k d) -> p k d", k=2), [bkk], ["va%d" % gp])
            return gg

        def prefix_state(pg):
            gp = pg % 2
            for j in range(2):
                for h in range(4):
                    state_update(gp, j, h, pg == 0 and j == 0)

        NPG = NG if stage >= 2 else 0
        for pg in range(NPG):
            gg_ = prefix_proj(pg)
            if pg >= 1:
                prefix_state(pg - 1)
            drain(gg_)

        def own_proj(g):
            gp = g % 2
            load_norm_transpose2(xo, g * 2)
            gg = gates_group(gp, False, False)
            adv(gg, 1)
            for c in range(4):
                def ev(bk, bkk, c=c):
                    S.add("act", lambda e: e.copy(out=qaT[gp][:, c, :], in_=bk[:, 0:GT]), reads=[bkk], writes=["qaT%d" % gp])
                fm_proj(lambda r, c=c: win[:, r, c * 128:(c + 1) * 128], WIN_K, ev)
            CP("pool", kaT[gp][:, :, 0:128], kaT[1 - gp][:, :, GT:GT + 128], ["kaT%d" % (1 - gp)], ["kaT%d" % gp])
            for kv in range(2):
                def ev(bk, bkk, kv=kv):
                    S.add("act", lambda e: e.copy(out=kaT[gp][:, kv, 128:128 + GT], in_=bk[:, 0:GT]), reads=[bkk], writes=["kaT%d" % gp])
                fm_proj(lambda r, kv=kv: wkd[:, r, kv * 128:(kv + 1) * 128], ["wkd"], ev)
            for c in range(8):
                conv_chunk(c, gp)
                adv(gg, 1)
            flush_silu()
            CP("pool", va[gp][:, 0, :, 0:64], va[1 - gp][:, 2, :, 0:64], ["va%d" % (1 - gp)], ["va%d" % gp])
            for j in range(2):
                bk, bkk = nextbank()
                for r in range(8):
                    MM(bk[:, 0:128], hT[:, r, j * 128:(j + 1) * 128], win[:, r, 640:768], r == 0, r == 7, WIN_K + ["hT"], [bkk])
                CP("dve", va[gp][:, 1 + j, :, 0:64], bk[:, 0:128].rearrange("p (k d) -> p k d", k=2), [bkk], ["va%d" % gp])
                bk, bkk = nextbank()
                for r in range(8):
                    MM(bk[:, :], hT[:, r, j * 128:(j + 1) * 128], win[:, r, 1792:2304], r == 0, r == 7, WIN_K + ["hT"], [bkk])
                CP("dve", vm[gp][:, j, :, 0:128], bk[:, :].rearrange("p (h d) -> p h d", h=4), [bkk], ["vm%d" % gp])
                bk, bkk = nextbank()
                for r in range(8):
                    MM(bk[:, :], hT[:, r, j * 128:(j + 1) * 128], win[:, r, 2304:2816], r == 0, r == 7, WIN_K + ["hT"], [bkk])
                ACT(so[gp][:, j, :], bk[:, :], AF.Sigmoid, [bkk], ["so%d" % gp])
                adv(gg, 2)
            return gg

        def mixX(g, j):
            gp = g % 2
            ti = g * 2 + j
            for kvg in range(2):
                for kb in range(2):
                    for hh in range(4):
                        hd = kvg * 4 + hh
                        ch, po = hd // 2, (hd % 2) * 64
                        sbk, sbkk = (PS_, "PS") if hh % 2 == 0 else (PV, "PV")
                        MM(sbk[:, (hh // 2) * 128:(hh // 2 + 1) * 128], kaT[gp][po:po + 64, kvg, (j + kb) * 128:(j + kb + 1) * 128],
                           qaT[gp][po:po + 64, ch, j * 128:(j + 1) * 128], True, True, ["kaT%d" % gp, "qaT%d" % gp], [sbkk])
                    for hh in range(4):
                        hd = kvg * 4 + hh
                        sbk, sbkk = (PS_, "PS") if hh % 2 == 0 else (PV, "PV")
                        ACT(PT[kb][:, hh * 128:(hh + 1) * 128], sbk[:, (hh // 2) * 128:(hh // 2 + 1) * 128], AF.Exp, [sbkk, "pp"], ["PT%d" % kb],
                            bias=pp[:, PP_AB + kb * 8 + hd:PP_AB + kb * 8 + hd + 1], scale=0.125)
                        yield
                    if kb == 0:
                        mk = cmb[:, 3, :] if ti == 0 else cmb[:, 1, :]
                    else:
                        mk = cmb[:, 2, :]
                    TT("dve", PT[kb][:].rearrange("p (h t) -> p h t", h=4), PT[kb][:].rearrange("p (h t) -> p h t", h=4),
                       mk.unsqueeze(1).to_broadcast([128, 4, 128]), ALU.mult, ["PT%d" % kb, "cmb"], ["PT%d" % kb])
                    yield
                for hh in range(4):
                    for kb in range(2):
                        MM(PV[:, hh * 65:(hh + 1) * 65], PT[kb][:, hh * 128:(hh + 1) * 128], va[gp][:, j + kb, kvg, :], kb == 0, kb == 1,
                           ["PT%d" % kb, "va%d" % gp], ["PV"])
                        yield
                pv3 = PV[:, 0:260].rearrange("p (h d) -> p h d", h=4)
                TT("dve", st8[:, 8:12], pv3[:, :, 64], sinkt[:, kvg * 4:kvg * 4 + 4], ALU.add, ["PV", "sinkt"], ["dn4"])
                yield
                RECIP(st8[:, 8:12], st8[:, 8:12], ["dn4"], ["dn4"])
                yield
                TT("dve", att_o[:, kvg * 256:(kvg + 1) * 256].rearrange("p (h d) -> p h d", h=4), pv3[:, :, 0:64],
                   st8[:, 8:12].unsqueeze(2).to_broadcast([128, 4, 64]), ALU.mult, ["PV", "dn4"], ["att_o"])
                yield
            mixp = mixb[ti % 2]
            mixk = "mix%d" % (ti % 2)
            ACT(junk[:, 0:512], att_o[:], AF.Square, ["att_o"], ["ssa"], accum=st8[:, 3:4])
            ACT(st8[:, 3:4], st8[:, 3:4], AF.Ln, ["ssa"], ["ssa"], bias=EPS, scale=1.0 / 512)
            ACT(st8[:, 3:4], st8[:, 3:4], AF.Exp, ["ssa"], ["ssa"], scale=-0.5)
            yield
            STT(mixp[:, 0:512], att_o[:], st8[:, 3:4], bc[:, G_AM:G_AM + 512], ALU.mult, ALU.mult, ["att_o", "ssa", "g_am"], [mixk])
            yield
            if dbg:
                DMA("sp", dbgd["d_att"][ti * 128:(ti + 1) * 128, :], att_o[:], ["att_o"], ["d_att"], "dbg")
            for h in range(4):
                pf = PF if h % 2 == 0 else PG
                pfk = "PF" if h % 2 == 0 else "PG"
                qT = qkT[gp][:, h, j * 128:(j + 1) * 128]
                kT = qkT[gp][:, 4 + h, j * 128:(j + 1) * 128]
                MM(pf[:, 0:128], kT, qT, True, True, ["qkT%d_%d" % (gp, 4 + h), "qkT%d_%d" % (gp, h)], [pfk])
                sm = smT[h % 2]
                smk = "smT%d" % (h % 2)
                STT(sm[:], pf[:, 0:128], gsc[gp][:, j, 0, h:h + 1], cmf[:, 2, :], ALU.mult, ALU.mult, [pfk, "gsc%d" % gp, "cmf"], [smk])
                yield
                MM(pf[:, 128:257], sm[:], vm[gp][:, j, h, :], True, False, [smk, "vm%d" % gp], [pfk])
                MM(pf[:, 128:257], qT, CNb[:, h, :], False, True, ["qkT%d_%d" % (gp, h), "CNb%d" % h], [pfk])
                yield
                TS("dve", st8[:, 1:2], pf[:, 256:257], -1.0, None, ALU.mult, None, [pfk], ["dn1"])
                TT("dve", st8[:, 1:2], st8[:, 1:2], pf[:, 256:257], ALU.max, [pfk, "dn1"], ["dn1"])
                yield
                TS("dve", st8[:, 1:2], st8[:, 1:2], gsc[gp][:, j, 1, h:h + 1], None, ALU.max, None, ["dn1", "gsc%d" % gp], ["dn1"])
                RECIP(st8[:, 1:2], st8[:, 1:2], ["dn1"], ["dn1"])
                yield
                STT(hmix[:, h * 128:(h + 1) * 128], pf[:, 128:256], st8[:, 1:2], so[gp][:, j, h * 128:(h + 1) * 128], ALU.mult, ALU.mult,
                    [pfk, "dn1", "so%d" % gp], ["hmix%d" % h])
                yield
                state_update(gp, j, h, False)
                yield
                ACT(junk[:, 0:128], hmix[:, h * 128:(h + 1) * 128], AF.Square, ["hmix%d" % h], ["ssh"], accum=st8[:, 2:3])
                ACT(st8[:, 2:3], st8[:, 2:3], AF.Ln, ["ssh"], ["ssh"], bias=EPS, scale=1.0 / 128)
                ACT(st8[:, 2:3], st8[:, 2:3], AF.Exp, ["ssh"], ["ssh"], scale=-0.5)
                yield
                STT(mixp[:, 512 + h * 128:512 + (h + 1) * 128],
                    hmix[:, h * 128:(h + 1) * 128], st8[:, 2:3],
                    bc[:, G_AM + 512 + h * 128:G_AM + 512 + (h + 1) * 128], ALU.mult, ALU.mult,
                    ["hmix%d" % h, "ssh", "g_am"], [mixk])
                yield
            if dbg:
                DMA("sp", dbgd["d_hm"][ti * 128:(ti + 1) * 128, :], hmix[:], ["hmix%d" % h for h in range(4)], ["d_hm"], "dbg")
                yield

        def mixY(g, j):
            gp = g % 2
            ti = g * 2 + j
            xrb = xr[ti % 2]
            xrk = "xr%d" % (ti % 2)
            if ti == 0:
                DMA("sp", xrb[:], xo[0:128, :], [], [xrk], xrk)
            if ti + 1 < NT:
                DMA("sp", xr[(ti + 1) % 2][:], xo[(ti + 1) * 128:(ti + 2) * 128, :], [], ["xr%d" % ((ti + 1) % 2)], "xr%d" % ((ti + 1) % 2))
            mT = tpA[ti % 2]
            mTk = "tpA%d" % (ti % 2)
            mixp = mixb[ti % 2]
            mixk = "mix%d" % (ti % 2)
            transpose8(mixp[:], mixk, mT[:], mTk)
            yield
            for nh in range(2):
                bk, bkk = nextbank()
                for r in range(8):
                    MM(bk[:, :], mT[:, r, :], wout[:, r, nh * 512:(nh + 1) * 512], r == 0, r == 7, [mTk] + WOUT_K, [bkk])
                TT("dve", xrb[:, nh * 512:(nh + 1) * 512], bk[:, :], xrb[:, nh * 512:(nh + 1) * 512], ALU.add, [bkk, xrk], [xrk])
                yield
            if dbg:
                DMA("sp", dbgd["d_x1"][ti * 128:(ti + 1) * 128, :], xrb[:], [xrk], ["d_x1"], "dbg")
            xcp = ybuf
            xck = "ybuf"
            norm_to_bf(xrb[:], [xrk], bc[:, G_CROSS:G_CROSS + 1024], "g_cross", xcp[:], xck)
            yield
            xcT = tpA[(ti + 1) % 2]
            xcTk = "tpA%d" % ((ti + 1) % 2)
            transpose8(xcp[:], xck, xcT[:], xcTk)
            yield
            bk, bkk = nextbank()
            for h in range(4):
                for r in range(8):
                    MM(bk[:, h * 128:(h + 1) * 128], wcq[:, r, h * 128:(h + 1) * 128], xcT[:, r, :], r == 0, r == 7, WCQ_K + [xcTk], [bkk])
            S.add("act", lambda e, bk=bk: e.copy(out=qcT[:].rearrange("p h t -> p (h t)"), in_=bk[:, :]), reads=[bkk], writes=["qcT"])
            yield
            for mt in range(2):
                pb = PA if mt == 0 else PB
                pbk = "PA" if mt == 0 else "PB"
                for h in range(4):
                    MM(pb[:, h * 128:(h + 1) * 128], KcT[:, h, mt * 128:(mt + 1) * 128], qcT[:, h, :], True, True, ["KcT", "qcT"], [pbk])
                ACT(PcT[mt][:], pb[:, :], AF.Exp, [pbk], ["PcT%d" % mt], scale=1.0 / math.sqrt(128.0))
            for h in range(4):
                pf = PA if h < 2 else PB
                pfk = "PA" if h < 2 else "PB"
                for mt in range(2):
                    MM(pf[:, (h % 2) * 129:(h % 2 + 1) * 129], PcT[mt][:, h * 128:(h + 1) * 128], Vc[:, mt, h, :], mt == 0, mt == 1, ["PcT%d" % mt, "Vc"], [pfk])
            for hp in range(2):
                pf = PA if hp == 0 else PB
                pfk = "PA" if hp == 0 else "PB"
                p3 = pf[:, 0:258].rearrange("p (h d) -> p h d", h=2)
                RECIP(st8[:, 12 + 2 * hp:12 + 2 * hp + 2], p3[:, :, 128], [pfk], ["dnc%d" % hp])
                for q in range(2):
                    h = hp * 2 + q
                    TS("dve", ocp[:, h * 128:(h + 1) * 128], p3[:, q, 0:128],
                       st8[:, 12 + h:13 + h], None, ALU.mult, None, [pfk, "dnc%d" % hp], ["ocp"])
            yield
            for r in range(4):
                TR(T1[:, 4 + r, :], ocp[:, r:512:4], identb, ["ocp", "cmb"], ["T1o"])
            S.add("act", lambda e: e.copy(out=ocT[:], in_=T1[:, 4:8, :]), reads=["T1o"], writes=["ocT"])
            yield
            for nh in range(2):
                bk, bkk = nextbank()
                for r in range(4):
                    MM(bk[:, :], ocT[:, r, :], wco[:, r, nh * 512:(nh + 1) * 512], r == 0, r == 3, ["ocT"] + WCO_K, [bkk])
                TT("dve", xrb[:, nh * 512:(nh + 1) * 512], bk[:, :], xrb[:, nh * 512:(nh + 1) * 512], ALU.add, [bkk, xrk], [xrk])
                yield
            DMA("sp", x2_d[ti * 128:(ti + 1) * 128, :], xrb[:], [xrk], ["x2d%d" % ti], "x2st%d" % (ti % 2))
            if dbg:
                DMA("sp", dbgd["d_x2"][ti * 128:(ti + 1) * 128, :], xrb[:], [xrk], ["d_x2"], "dbg")
            xnp = ybuf
            xnk = "ybuf"
            norm_to_bf(xrb[:], [xrk], bc[:, G_FFN:G_FFN + 1024], "g_ffn", xnp[:], xnk)
            yield
            xnT = tpA[ti % 2]
            xnTk = "tpA%d" % (ti % 2)
            transpose8(xnp[:], xnk, xnT[:], xnTk)
            yield
            bk, bkk = nextbank()
            for r in range(8):
                MM(bk[:, 0:36], xnT[:, r, :], wr[:, r, :], r == 0, r == 7, [xnTk, "wr"], [bkk])
            TT("dve", lg[:], bk[:, 0:36], bc[:, MISC:MISC + 36], ALU.add, [bkk, "misc"], ["lg"])
            yield
            R_GMAX, R_NG, R_GSUM, R_GOH, R_PEN, R_T8, R_D, R_P2, R_D1 = 0, 1, 2, 4, 8, 16, 24, 25, 26
            S.add("dve", lambda e: e.tensor_reduce(out=rt[:, R_GMAX:R_GMAX + 1], in_=lg[:, 0:4], axis=AX.X, op=ALU.max), reads=["lg"], writes=["rt"])
            TS("dve", rt[:, R_NG:R_NG + 1], rt[:, R_GMAX:R_GMAX + 1], -1.0, None, ALU.mult, None, ["rt"], ["rt"])
            yield
            ACT(rt[:, R_GOH:R_GOH + 4], lg[:, 0:4], AF.Exp, ["lg", "rt"], ["rt"], bias=rt[:, R_NG:R_NG + 1], accum=rt[:, R_GSUM:R_GSUM + 1])
            yield
            RECIP(rt[:, R_GSUM:R_GSUM + 1], rt[:, R_GSUM:R_GSUM + 1], ["rt"], ["rt"])
            yield
            TS("dve", rt[:, R_GOH:R_GOH + 4], lg[:, 0:4], rt[:, R_GMAX:R_GMAX + 1], None, ALU.is_equal, None, ["lg", "rt"], ["rt"])
            TS("dve", rt[:, R_PEN:R_PEN + 4], rt[:, R_GOH:R_GOH + 4], 1e30, -1e30, ALU.mult, ALU.add, ["rt"], ["rt"])
            TT("dve", cbuf[:].rearrange("p (g e) -> p g e", g=4), lg[:, 4:36].rearrange("p (g e) -> p g e", g=4),
               rt[:, R_PEN:R_PEN + 4].unsqueeze(2).to_broadcast([128, 4, 8]), ALU.add, ["lg", "rt"], ["cbuf"])
            yield
            S.add("dve", lambda e: e.max(out=rt[:, R_T8:R_T8 + 8], in_=cbuf[:]), reads=["cbuf"], writes=["rt"])
            yield
            TS("dve", A1[:], cbuf[:], rt[:, R_T8:R_T8 + 1], None, ALU.is_equal, None, ["cbuf", "rt"], ["A1"])
            TS("dve", A2[:], cbuf[:], rt[:, R_T8 + 1:R_T8 + 2], None, ALU.is_equal, None, ["cbuf", "rt"], ["A2"])
            yield
            TT("dve", rt[:, R_D:R_D + 1], rt[:, R_T8 + 1:R_T8 + 2], rt[:, R_T8:R_T8 + 1], ALU.subtract, ["rt"], ["rt"])
            ACT(rt[:, R_P2:R_P2 + 1], rt[:, R_D:R_D + 1], AF.Sigmoid, ["rt"], ["rt"])
            yield
            TT("dve", gt[:, ti, 1:2], rt[:, R_P2:R_P2 + 1], rt[:, R_GSUM:R_GSUM + 1], ALU.mult, ["rt"], ["gt"])
            TT("dve", gt[:, ti, 0:1], rt[:, R_GSUM:R_GSUM + 1], gt[:, ti, 1:2], ALU.subtract, ["rt", "gt"], ["gt"])
            TT("dve", Ab[:], A1[:], A2[:], ALU.add, ["A1", "A2"], ["Ab"])
            yield
            bk, bkk = nextbank()
            MM(bk[:, 0:NE], cmb[:, 4, :], Ab[:], True, False, ["cmb", "Ab"], [bkk])
            MM(bk[:, 0:NE], ones_b[:], Acum[:], False, True, ["ones_b", "Acum"], [bkk])
            TT("dve", cbuf[:], bk[:, 0:NE], pp[:, PP_BASE:PP_BASE + NE], ALU.add, [bkk, "pp", "A1", "A2"], ["cbuf"])
            yield
            TT("pool", Acum[:], Acum[:], Ab[:], ALU.add, ["Acum", "Ab"], ["Acum"])
            STT(junk[:, 0:NE], A1[:], 1.0, cbuf[:], ALU.mult, ALU.mult, ["A1", "cbuf"], ["rt"], accum=rt[:, R_D1:R_D1 + 1])
            STT(junk[:, 0:NE], A2[:], 1.0, cbuf[:], ALU.mult, ALU.mult, ["A2", "cbuf"], ["rt"], accum=rt[:, R_D1 + 1:R_D1 + 2])
            yield
            TS("dve", rt[:, R_D1:R_D1 + 2], rt[:, R_D1:R_D1 + 2], float(NE * CAP), None, ALU.min, None, ["rt"], ["rt"])
            CP("dve", di[:, ti, :], rt[:, R_D1:R_D1 + 2], ["rt"], ["di"])
            yield
            for k in range(2 if stage >= 4 else 0):
                S.add("pool", lambda e, k=k, xnp=xnp, ti=ti: e.indirect_dma_start(out=xs_d, out_offset=bass.IndirectOffsetOnAxis(ap=di[:, ti, k:k + 1], axis=0),
                                                                     in_=xnp[:, :], in_offset=None, bounds_check=breg(e), oob_is_err=False),
                      reads=["di", xnk, "xs_d"], writes=["xs_s%d" % k], dma="scat%d" % k)
            if dbg:
                DMA("sp", dbgd["d_lg"][ti * 128:(ti + 1) * 128, :], lg[:], ["lg"], ["d_lg"], "dbg")
                DMA("sp", dbgd["d_gt"][ti * 128:(ti + 1) * 128, :], gt[:, ti, :], ["gt"], ["d_gt"], "dbg")
                DMA("sp", dbgd["d_di"][ti * 128:(ti + 1) * 128, :], rt[:, R_D1:R_D1 + 2], ["rt"], ["d_di"], "dbg")

                yield

        def interleave(gens):
            gens = [g_ for g_ in gens if g_ is not None]
            while gens:
                for g_ in list(gens):
                    try:
                        next(g_)
                    except StopIteration:
                        gens.remove(g_)


        if stage >= 3:
            drain(own_proj(0))
            if NPG > 0:
                prefix_state(NPG - 1)
            gg_ = own_proj(1) if NG > 1 else None
            interleave([mixX(0, 0), gg_])
            for t in range(NT):
                gg_ = None
                if (t + 1) % 2 == 0 and (t + 1) // 2 + 1 < NG:
                    gg_ = own_proj((t + 1) // 2 + 1)
                interleave([mixX((t + 1) // 2, (t + 1) % 2) if t + 1 < NT else None, mixY(t // 2, t % 2), gg_])
        elif NPG > 0:
            prefix_state(NPG - 1)

        PH2_W = WIN_K + WOUT_K + WCQ_K + WCO_K + ["wkd"]
        DMA("sp", bc[:, G_MIX:G_MIX + 1024], bcd[:, 4096:5120], [], ["g_mix"], "bc0")
        NSLOT = 3
        NE_run = int(os.environ.get('MOE_N', NE)) if stage >= 5 else 0
        blocks = [(e_, b) for e_ in range(NE_run) for b in range(CAP // 128)]
        NB = len(blocks)
        ocp2 = [ocp, ocpB]
        ocT2 = [ocT, ocTB]

        def wkeys(e_):
            s_ = e_ % NSLOT
            return ["eg%d" % s_], ["eu%d" % s_], ["ed%d" % s_]

        def wviews(e_):
            base = (e_ % NSLOT) * 12288
            return (arena[:, base:base + 4096].rearrange("p (r c) -> p r c", r=8),
                    arena[:, base + 4096:base + 8192].rearrange("p (r c) -> p r c", r=8),
                    arena[:, base + 8192:base + 12288].rearrange("p (r c) -> p r c", r=4))

        def issue_w(e_):
            s_ = e_ % NSLOT
            base = s_ * 12288
            gk, uk, dk = wkeys(e_)
            extra = PH2_W if e_ < NSLOT else []
            DMA("pool", arena[:, base:base + 4096].rearrange("p (a c) -> p a c", a=2),
                w_eg[e_].rearrange("(p r) c -> p (r c)", r=8).rearrange("p (a c) -> p a c", a=2), [], gk + extra, "eg%d" % s_)
            DMA("pool", arena[:, base + 4096:base + 8192].rearrange("p (a c) -> p a c", a=2),
                w_eu[e_].rearrange("(p r) c -> p (r c)", r=8).rearrange("p (a c) -> p a c", a=2), [], uk + extra, "eu%d" % s_)
            DMA("pool", arena[:, base + 8192:base + 12288].rearrange("p (a c) -> p a c", a=2),
                w_ed[e_].rearrange("(p r) c -> p (r c)", r=4).rearrange("p (a c) -> p a c", a=2), [], dk + extra, "ed%d" % s_)

        def stA(bi):
            e_, b = blocks[bi]
            row0 = e_ * CAP + b * 128
            xg = bfA[bi % 3]
            xgk = "bfA%d" % (bi % 3)
            DMA("sp", xg[:], xs_d[row0:row0 + 128, :], ["xs_d", "xs_s0", "xs_s1"], [xgk], xgk)
            transpose8(xg[:], xgk, tpA[bi % 2][:], "tpA%d" % (bi % 2))

        def stB(bi):
            e_, b = blocks[bi]
            gk, uk, dk = wkeys(e_)
            wg_, wu_, wd_ = wviews(e_)
            xgT = tpA[bi % 2]
            xgTk = "tpA%d" % (bi % 2)
            for r in range(8):
                MM(PA[:, :], xgT[:, r, :], wg_[:, r, :], r == 0, r == 7, [xgTk] + gk, ["PA"])
            for r in range(8):
                MM(PB[:, :], xgT[:, r, :], wu_[:, r, :], r == 0, r == 7, [xgTk] + uk, ["PB"])
            ACT(att_o[:], PA[:, :], AF.Silu, ["PA"], ["att_o"])
            TT("dve", ocp2[bi % 2][:], att_o[:], PB[:, :], ALU.mult,
               ["att_o", "PB"], ["ocp%d" % (bi % 2)])

        def stC1(bi):
            oc = ocp2[bi % 2]
            ot = ocT2[bi % 2]
            for r in range(4):
                TR(T1[:, 4 + r, :], oc[:, r:512:4], identb, ["ocp%d" % (bi % 2), "cmb"], ["T1o"])
            S.add("act", lambda e: e.copy(out=ot[:], in_=T1[:, 4:8, :]), reads=["T1o"], writes=["ocT%d" % (bi % 2)])

        def stC2(bi):
            e_, b = blocks[bi]
            row0 = e_ * CAP + b * 128
            gk, uk, dk = wkeys(e_)
            wg_, wu_, wd_ = wviews(e_)
            ot = ocT2[bi % 2]
            yb = xt[bi % 2]
            ybk = "xt%d" % (bi % 2)
            for nh in range(2):
                pb = PS_ if nh == 0 else PV
                pbk = "PS" if nh == 0 else "PV"
                for r in range(4):
                    MM(pb[:, :], ot[:, r, :], wd_[:, r, nh * 512:(nh + 1) * 512], r == 0, r == 3, ["ocT%d" % (bi % 2)] + dk, [pbk])
                if nh == 0:
                    S.add("act", lambda e, yb=yb, pb=pb: e.copy(out=yb[:, 0:512], in_=pb[:, :]), reads=[pbk], writes=[ybk])
                else:
                    CP("dve", yb[:, 512:1024], pb[:, :], [pbk], [ybk])
            DMA("sp", ys_d[row0:row0 + 128, :], yb[:], [ybk], ["ys_d"], "yst%d" % (bi % 2))
            if b == CAP // 128 - 1 and e_ + NSLOT < NE_run:
                issue_w(e_ + NSLOT)

        for e_ in range(min(NSLOT, NE_run)):
            issue_w(e_)
        if NB > 0:
            stA(0)
        if NB > 1:
            stA(1)
        if NB > 0:
            stB(0)
        for bi in range(NB):
            if bi + 2 < NB:
                stA(bi + 2)
            if bi >= 1:
                stC2(bi - 1)
            if bi + 1 < NB:
                stB(bi + 1)
            stC1(bi)
        if NB > 0:
            stC2(NB - 1)

        def comb_bufs(ti):
            y1, y1k = (xt[0], "xt0") if ti % 2 == 0 else (bc[:, G_CROSS:G_CROSS + 1024], "g_cross")
            y2, y2k = (xt[1], "xt1") if ti % 2 == 0 else (bc[:, G_FFN:G_FFN + 1024], "g_ffn")
            return y1, y1k, y2, y2k

        def comb_fetch(ti):
            xrb = xr[ti % 2]
            xrk = "xr%d" % (ti % 2)
            DMA("sp", xrb[:], x2_d[ti * 128:(ti + 1) * 128, :], ["x2d%d" % ti], [xrk], xrk)
            y1, y1k, y2, y2k = comb_bufs(ti)
            igA = ig0 if ti % 2 == 0 else ig2
            igB = ig1 if ti % 2 == 0 else ig3
            igAk = "ig0" if ti % 2 == 0 else "ig2"
            igBk = "ig1" if ti % 2 == 0 else "ig3"
            CP("dve", igA[:], di[:, ti, 0:1], ["di"], [igAk])
            CP("dve", igB[:], di[:, ti, 1:2], ["di"], [igBk])
            S.add("pool", lambda e, y1=y1, igA=igA: e.indirect_dma_start(out=y1[:, :], out_offset=None, in_=ys_d,
                                                         in_offset=bass.IndirectOffsetOnAxis(ap=igA[:, :], axis=0), bounds_check=breg(e), oob_is_err=False),
                  reads=[igAk, "ys_d"], writes=[y1k], dma=y1k)
            S.add("pool", lambda e, y2=y2, igB=igB: e.indirect_dma_start(out=y2[:, :], out_offset=None, in_=ys_d,
                                                         in_offset=bass.IndirectOffsetOnAxis(ap=igB[:, :], axis=0), bounds_check=breg(e), oob_is_err=False),
                  reads=[igBk, "ys_d"], writes=[y2k], dma=y2k)

        def comb_compute(ti):
            xrb = xr[ti % 2]
            xrk = "xr%d" % (ti % 2)
            y1, y1k, y2, y2k = comb_bufs(ti)
            STT(xrb[:], y1[:], gt[:, ti, 0:1], xrb[:], ALU.mult, ALU.add, [y1k, "gt", xrk], [xrk])
            STT(xrb[:], y2[:], gt[:, ti, 1:2], xrb[:], ALU.mult, ALU.add, [y2k, "gt", xrk], [xrk])
            ACT(junk[:], xrb[:], AF.Square, [xrk], ["ss"], accum=st8[:, 0:1])
            rstd_from_ss(0, 1024, ["ss"], ["ss"])
            STT(xrb[:], xrb[:], st8[:, 0:1], bc[:, G_MIX:G_MIX + 1024], ALU.mult, ALU.mult, [xrk, "ss", "g_mix"], [xrk])
            DMA("sp", outd[ti * 128:(ti + 1) * 128, :], xrb[:], [xrk], ["out%d" % ti], "ost%d" % (ti % 2))

        NCT = NT if stage >= 6 else 0
        if NCT > 0:
            comb_fetch(0)
        for ti in range(NCT):
            if ti + 1 < NCT:
                comb_fetch(ti + 1)
            comb_compute(ti)
        fin = ["out%d" % ti for ti in range(NT if stage >= 6 else 0)] + ["KcT", "Vc", "CN0", "CNb3"]
        if dbg:
            fin += list(dbgd.keys())
        S.add("sp", None, reads=fin)

        S.finalize()
        sems = {e: [es.enter_context(nc.semaphore("sem_%s_%d" % (e, i))) for i in range(S.nsig[e] // EPOCH + 1)] for e in ENGS}
        dma_sems = {k: es.enter_context(nc.semaphore("ds%d" % i)) for i, k in enumerate(S.dma_cnt)}
        with nc.Block() as block:
            @block.sync
            def _(e):
                S.emit_engine("sp", e, sems, dma_sems)
                for k_, c_ in S.dma_cnt.items():
                    e.wait_ge(dma_sems[k_], c_)

            @block.scalar
            def _(e):
                S.emit_engine("act", e, sems, dma_sems)

            @block.vector
            def _(e):
                S.emit_engine("dve", e, sems, dma_sems)

            @block.gpsimd
            def _(e):
                S.emit_engine("pool", e, sems, dma_sems)

            @block.tensor
            def _(e):
                S.emit_engine("pe", e, sems, dma_sems)
    return nc, S


def host_inputs(inp):
    f = np.float32
    x = np.asarray(inp["x"], f)
    mem = np.asarray(inp["mem"], f)
    bcrow = np.concatenate([
        np.asarray(inp["norm_mix"], f)[0], np.asarray(inp["norm_cross"], f)[0], np.asarray(inp["norm_ffn"], f)[0],
        np.concatenate([np.asarray(inp["norm_att_out"], f)[0], np.asarray(inp["norm_ml_out"], f)[0]]),
        np.asarray(inp["norm_final"], f), np.asarray(inp["norm_mem"], f)[0],
        np.asarray(inp["b_router_group"], f)[0], np.asarray(inp["b_router_expert"], f)[0], np.asarray(inp["att_sinks"], f)[0],
        np.zeros(128 - 44, f)])
    bcd = np.ascontiguousarray(np.broadcast_to(bcrow[None, :], (128, bcrow.shape[0])))
    w_r = np.ascontiguousarray(np.concatenate([np.asarray(inp["w_router_group"], f)[0], np.asarray(inp["w_router_expert"], f)[0]], axis=1))
    s_ = np.arange(128)[:, None]
    t_ = np.arange(128)[None, :]
    m_prev = (s_ > t_).astype(f)
    m_cur = (s_ <= t_).astype(f)
    tri_lt = (s_ < t_).astype(f)
    slopes = np.exp2(-8.0 * np.arange(1, 9, dtype=np.float64) / 8).astype(f)
    common = dict(
        w_in=np.ascontiguousarray(np.asarray(inp["w_in"], f)[0]), w_out=np.ascontiguousarray(np.asarray(inp["w_out"], f)[0]),
        w_cq=np.ascontiguousarray(np.asarray(inp["w_cq"], f)[0]), w_ckv=np.ascontiguousarray(np.asarray(inp["w_ckv"], f)[0]),
        w_co=np.ascontiguousarray(np.asarray(inp["w_co"], f)[0]), w_r=w_r,
        w_eg=np.ascontiguousarray(np.asarray(inp["w_e_gate"], f)[0]), w_eu=np.ascontiguousarray(np.asarray(inp["w_e_up"], f)[0]),
        w_ed=np.ascontiguousarray(np.asarray(inp["w_e_down"], f)[0]), bcd=bcd)
    conv_w = np.asarray(inp["conv_w"], f)[0]
    conv_b = np.asarray(inp["conv_b"], f)[0]
    b_g = np.asarray(inp["b_gates"], f)[0]
    maps = []
    for c in range(8):
        b, half = c // 2, c % 2
        pp = np.zeros((128, 128), f)
        pp[:, 0:32] = conv_w.reshape(4, 8, 128).transpose(2, 1, 0).reshape(128, 32)
        pp[:, 32:40] = conv_b.reshape(8, 128).T
        pp[0:4, 40] = b_g[0:4]
        pp[0:4, 41] = b_g[4:8]
        pp[:, 42] = 1.0 if half == 1 else 0.0
        pp[:, 43] = 0.0 if half == 1 else -30000.0
        for kb in range(2):
            for h in range(8):
                pp[:, 44 + kb * 8 + h] = slopes[h] * (np.arange(128) + kb * 128 - 255.0)
        for h in range(8):
            pp[:, 60 + h] = slopes[h] * (np.arange(128) + 128 - 255.0)
        pp[:, 68:100] = (np.arange(32) * CAP)[None, :]
        cm = np.zeros((128, 5, 128), f)
        cm[:, 0] = np.eye(128, dtype=f)
        cm[:, 1] = m_prev
        cm[:, 2] = m_cur
        cm[:, 3] = m_prev if half == 1 else 0.0
        cm[:, 4] = tri_lt
        d = dict(common)
        d["xo"] = np.ascontiguousarray(x[b, half * 2048:(half + 1) * 2048])
        d["xp"] = np.ascontiguousarray(x[b, 0:2048]) if half == 1 else np.zeros((2048, 1024), f)
        d["memb"] = np.ascontiguousarray(mem[b])
        d["ppd"] = pp
        d["cmd"] = cm
        maps.append(d)
    return maps


_NC_CACHE = {}


def kernel(**inputs):
    if "nc" not in _NC_CACHE:
        _NC_CACHE["nc"] = build(False)[0]
    nc = _NC_CACHE["nc"]
    maps = host_inputs(inputs)
    res = run_bass_kernel_spmd(nc, maps, core_ids=list(range(8)))
    out = np.zeros((4, 4096, 1024), np.float32)
    for c in range(8):
        b, half = c // 2, c % 2
        out[b, half * 2048:(half + 1) * 2048] = res.results[c]["out"]
    return out
```
